# Optimizing a Trainium2 kernel written in Bass

```python
import math
import jax, jax.numpy as jnp
from jax import lax
import numpy as np

D_MODEL = 2048
BATCH = 2
SEQ = 8192
DEPTH = 4

GRID_W = 64
CTX_LEN = 256
NORM_EPS = 1e-6
N_MOD = 6

RW_HEADS = 8
RW_HEAD_DIM = 64
RW_WIDTH = RW_HEADS * RW_HEAD_DIM
RW_DECAY_RANK = 64
RW_AAA_RANK = 64
RW_GATE_RANK = 128
RW_LN_EPS = 64e-5

S5_WIDTH = 512
S5_GROUP = 16
S5_GROUPS = S5_WIDTH // S5_GROUP
S5_STATE = 64
S5_DT_MIN = 0.001
S5_DT_MAX = 0.1

AT_HEADS = 8
AT_KV_HEADS = 2
AT_GROUP = AT_HEADS // AT_KV_HEADS
AT_HEAD_DIM = 128
AT_WIDTH = AT_HEADS * AT_HEAD_DIM
AT_KV_WIDTH = AT_KV_HEADS * AT_HEAD_DIM
Q_BLOCK = 128
ROPE_THETA = 10000.0
ROPE_AXIS_DIM = AT_HEAD_DIM // 2

N_BRANCH = 3
MIX_WIDTH = RW_WIDTH + S5_WIDTH + AT_WIDTH

OFF_WD = 3 * RW_WIDTH
OFF_AD = OFF_WD + 2 * RW_DECAY_RANK
OFF_GD = OFF_AD + 2 * RW_AAA_RANK
OFF_S5 = OFF_GD + RW_GATE_RANK
OFF_Q = OFF_S5 + S5_WIDTH
OFF_K = OFF_Q + AT_WIDTH
OFF_V = OFF_K + AT_KV_WIDTH
OFF_GATE = OFF_V + AT_KV_WIDTH
N_IN = OFF_GATE + N_BRANCH * D_MODEL

N_EXPERTS = 16
EXPERT_FF = 1024
CAPACITY_FACTOR = 2

kernel_name = "hybrid_rwkv7_s5_gqa_ecmoe_diffusion_trunk"

F32 = jnp.float32


def rms_norm(x, g):
    x32 = x.astype(F32)
    y = x32 * lax.rsqrt(jnp.mean(x32 * x32, axis=-1, keepdims=True) + NORM_EPS)
    return (y * g.astype(F32)).astype(x.dtype)


def modulate(h, shift, scale):
    return h * (1 + scale) + shift


def short_conv(x, w):
    xp = jnp.pad(x, ((0, 0), (1, 1), (0, 0)))
    return xp[:, :-2] * w[0] + xp[:, 1:-1] * w[1] + xp[:, 2:] * w[2]


def rwkv_scan(state0, decay, key, value, kk, a, r=None, reverse=False):
    emit = r is not None
    seqs = (decay, key, value, kk, a) + ((r,) if emit else ())
    xs = tuple(jnp.moveaxis(t, 1, 0) for t in seqs)

    def step(S, inp):
        w_t, k_t, v_t, kk_t, a_t = inp[:5]
        S = (S * w_t[:, :, None, :]
             - jnp.einsum('bhij,bhj->bhi', S, kk_t)[..., None] * (kk_t * a_t)[:, :, None, :]
             + v_t[..., None] * k_t[:, :, None, :])
        out = jnp.einsum('bhij,bhj->bhi', S, inp[5]) if emit else None
        return S, out

    S, ys = lax.scan(step, state0, xs, reverse=reverse)
    return S, (jnp.moveaxis(ys, 0, 1) if emit else None)


def rwkv_prep(p, conv_w, w0, w2, a0, a2, k_k, k_a):
    B, L, _ = p.shape

    def heads(t):
        return t.reshape(B, L, RW_HEADS, RW_HEAD_DIM).astype(F32)

    rkv = short_conv(p[..., :OFF_WD], conv_w)
    r, k, v = jnp.split(rkv, 3, axis=-1)
    wd = jnp.tanh(p[..., OFF_WD:OFF_AD]).reshape(B, L, 2, RW_DECAY_RANK)
    ad = p[..., OFF_AD:OFF_GD].reshape(B, L, 2, RW_AAA_RANK)
    w_log = -jax.nn.softplus(-(w0 + jnp.einsum('blzr,zrc->blzc', wd, w2)).astype(F32)) - 0.5
    decay = jnp.exp(-jnp.exp(w_log))
    a = jax.nn.sigmoid((a0 + jnp.einsum('blzr,zrc->blzc', ad, a2)).astype(F32))
    kk = heads(k * k_k)
    kk = kk * lax.rsqrt(jnp.sum(kk * kk, axis=-1, keepdims=True) + 1e-12)
    kd = k.astype(F32)[:, :, None] * (1 + (a - 1) * k_a.astype(F32))
    dirs = [(heads(decay[:, :, z]), heads(kd[:, :, z]), heads(a[:, :, z])) for z in range(2)]
    return heads(r), heads(v), kk, dirs


def rwkv_mix(pc, pl, conv_w, w0, w2, a0, a2, g2, k_k, k_a, r_k, ln_g, ln_b, need_ctx):
    r_c, v_c, kk_c, dirs_c = rwkv_prep(pc, conv_w, w0, w2, a0, a2, k_k, k_a)
    r_l, v_l, kk_l, dirs_l = rwkv_prep(pl, conv_w, w0, w2, a0, a2, k_k, k_a)
    B = pl.shape[0]
    o_c, o_l = 0.0, 0.0
    for z in range(2):
        rev = z == 1
        S0 = jnp.zeros((B, RW_HEADS, RW_HEAD_DIM, RW_HEAD_DIM), F32)
        w_cz, k_cz, a_cz = dirs_c[z]
        S_c, y_c = rwkv_scan(S0, w_cz, k_cz, v_c, kk_c, a_cz, r_c if need_ctx else None, rev)
        w_lz, k_lz, a_lz = dirs_l[z]
        _, y_l = rwkv_scan(S_c, w_lz, k_lz, v_l, kk_l, a_lz, r_l, rev)
        o_l = o_l + y_l
        if need_ctx:
            o_c = o_c + y_c

    def finish(o, r, v, dirs, p):
        B_, L_ = o.shape[:2]
        mu = jnp.mean(o, axis=-1, keepdims=True)
        var = jnp.mean(jnp.square(o - mu), axis=-1, keepdims=True)
        o_n = ((o - mu) * lax.rsqrt(var + RW_LN_EPS)).reshape(B_, L_, RW_WIDTH)
        bonus = jnp.sum(r * (dirs[0][1] + dirs[1][1]) * r_k.astype(F32), axis=-1, keepdims=True) * v
        y = o_n * ln_g + ln_b + bonus.reshape(B_, L_, RW_WIDTH)
        g = jax.nn.sigmoid(p[..., OFF_GD:OFF_S5]) @ g2
        return (y * g).astype(p.dtype)

    y_l = finish(o_l, r_l, v_l, dirs_l, pl)
    y_c = finish(o_c, r_c, v_c, dirs_c, pc) if need_ctx else None
    return y_c, y_l


def s5_discretize(lam_re, lam_im, log_dt, b_re, b_im):
    lam_re, lam_im = lam_re.astype(F32), lam_im.astype(F32)
    dt = jnp.exp(log_dt.astype(F32))[:, None]
    mag = jnp.exp(lam_re * dt)
    ab_re, ab_im = mag * jnp.cos(lam_im * dt), mag * jnp.sin(lam_im * dt)
    den = lam_re * lam_re + lam_im * lam_im
    num_re = ab_re - 1.0
    co_re = (num_re * lam_re + ab_im * lam_im) / den
    co_im = (ab_im * lam_re - num_re * lam_im) / den
    b_re, b_im = b_re.astype(F32), b_im.astype(F32)
    bb_re = co_re[..., None] * b_re - co_im[..., None] * b_im
    bb_im = co_re[..., None] * b_im + co_im[..., None] * b_re
    return ab_re, ab_im, bb_re, bb_im


def complex_scan(ab_re, ab_im, bu_re, bu_im, reverse):
    L = bu_re.shape[0]
    a_re = jnp.broadcast_to(ab_re, (L, 1) + ab_re.shape)
    a_im = jnp.broadcast_to(ab_im, (L, 1) + ab_im.shape)

    def op(e1, e2):
        a1r, a1i, b1r, b1i = e1
        a2r, a2i, b2r, b2i = e2
        return (a2r * a1r - a2i * a1i, a2r * a1i + a2i * a1r,
                a2r * b1r - a2i * b1i + b2r, a2r * b1i + a2i * b1r + b2i)

    _, _, xr, xi = lax.associative_scan(op, (a_re, a_im, bu_re, bu_im), reverse=reverse)
    return xr, xi


def s5_readout(xr, xi, c_re, c_im):
    return jnp.einsum('lbgn,gcn->lbgc', xr, c_re) - jnp.einsum('lbgn,gcn->lbgc', xi, c_im)


def s5_mix(pc, pl, lam_re, lam_im, log_dt, b_re, b_im, c_re, c_im, d_skip, w_glu, need_ctx):
    def time_major(p):
        B, L, _ = p.shape
        u = p[..., OFF_S5:OFF_Q].astype(F32).reshape(B, L, S5_GROUPS, S5_GROUP)
        return jnp.moveaxis(u, 1, 0)

    uc, ul = time_major(pc), time_major(pl)
    y_c, y_l = 0.0, 0.0
    for z in range(2):
        rev = z == 1
        ab_re, ab_im, bb_re, bb_im = s5_discretize(lam_re[z], lam_im[z], log_dt[z], b_re[z], b_im[z])
        cr, ci = c_re[z].astype(F32), c_im[z].astype(F32)
        xc_re, xc_im = complex_scan(ab_re, ab_im,
                                    jnp.einsum('lbgc,gnc->lbgn', uc, bb_re),
                                    jnp.einsum('lbgc,gnc->lbgn', uc, bb_im), rev)
        end = 0 if rev else -1
        h_re, h_im = xc_re[end], xc_im[end]
        start = -1 if rev else 0
        lr = jnp.einsum('lbgc,gnc->lbgn', ul, bb_re).at[start].add(ab_re * h_re - ab_im * h_im)
        li = jnp.einsum('lbgc,gnc->lbgn', ul, bb_im).at[start].add(ab_re * h_im + ab_im * h_re)
        xl_re, xl_im = complex_scan(ab_re, ab_im, lr, li, rev)
        y_l = y_l + s5_readout(xl_re, xl_im, cr, ci)
        if need_ctx:
            y_c = y_c + s5_readout(xc_re, xc_im, cr, ci)

    def finish(y, u, dtype):
        L, B = y.shape[:2]
        y = y + d_skip.astype(F32).reshape(S5_GROUPS, S5_GROUP) * u
        y = jax.nn.gelu(jnp.moveaxis(y, 0, 1).reshape(B, L, S5_WIDTH))
        return (y * jax.nn.sigmoid(y @ w_glu.astype(F32))).astype(dtype)

    out_l = finish(y_l, ul, pl.dtype)
    out_c = finish(y_c, uc, pc.dtype) if need_ctx else None
    return out_c, out_l


def rope_axis(x, ang):
    half = x.shape[-1] // 2
    shape = (1, ang.shape[0]) + (1,) * (x.ndim - 3) + (ang.shape[1],)
    cos = jnp.cos(ang).reshape(shape).astype(x.dtype)
    sin = jnp.sin(ang).reshape(shape).astype(x.dtype)
    x1, x2 = x[..., :half], x[..., half:]
    return jnp.concatenate([x1 * cos - x2 * sin, x2 * cos + x1 * sin], axis=-1)


def rope_2d(x, ang_row, ang_col):
    h = x.shape[-1] // 2
    return jnp.concatenate([rope_axis(x[..., :h], ang_row), rope_axis(x[..., h:], ang_col)], axis=-1)


def attend(q, k, v):
    s = jnp.einsum('bqkgd,bskd->bkgqs', q, k).astype(F32) * (AT_HEAD_DIM ** -0.5)
    p = jax.nn.softmax(s, axis=-1).astype(v.dtype)
    return jnp.einsum('bkgqs,bskd->bqkgd', p, v)


def attn_mix(pc, pl, qn, kn, ang_row, ang_col, need_ctx):
    def kv(p):
        B, L, _ = p.shape
        k = rms_norm(p[..., OFF_K:OFF_V].reshape(B, L, AT_KV_HEADS, AT_HEAD_DIM), kn)
        v = p[..., OFF_V:OFF_GATE].reshape(B, L, AT_KV_HEADS, AT_HEAD_DIM)
        return k, v

    def queries(p):
        B, L, _ = p.shape
        return rms_norm(p[..., OFF_Q:OFF_K].reshape(B, L, AT_KV_HEADS, AT_GROUP, AT_HEAD_DIM), qn)

    B, L, _ = pl.shape
    k_c, v_c = kv(pc)
    k_l, v_l = kv(pl)
    k_l = rope_2d(k_l, ang_row, ang_col)
    q_l = rope_2d(queries(pl), ang_row, ang_col)
    k_all = jnp.concatenate([k_c, k_l], axis=1)
    v_all = jnp.concatenate([v_c, v_l], axis=1)
    nb = L // Q_BLOCK
    qb = jnp.moveaxis(q_l.reshape(B, nb, Q_BLOCK, AT_KV_HEADS, AT_GROUP, AT_HEAD_DIM), 1, 0)
    o = lax.map(lambda qq: attend(qq, k_all, v_all), qb)
    o_l = jnp.moveaxis(o, 0, 1).reshape(B, L, AT_WIDTH)
    o_c = attend(queries(pc), k_c, v_c).reshape(pc.shape[0], pc.shape[1], AT_WIDTH) if need_ctx else None
    return o_c, o_l


def merge(p, y_rw, y_s5, y_at, w_branch, w_out):
    B, L, _ = p.shape
    g = jax.nn.sigmoid(p[..., OFF_GATE:]).reshape(B, L, N_BRANCH, D_MODEL)
    m = (g[:, :, 0] * (y_rw @ w_branch[:RW_WIDTH])
         + g[:, :, 1] * (y_s5 @ w_branch[RW_WIDTH:RW_WIDTH + S5_WIDTH])
         + g[:, :, 2] * (y_at @ w_branch[RW_WIDTH + S5_WIDTH:]))
    return m @ w_out


def ec_moe(h, router, wg, wu, wd):
    B, n, D = h.shape
    cap = CAPACITY_FACTOR * n // N_EXPERTS
    aff = jax.nn.softmax((h @ router).astype(F32), axis=-1)
    gate, idx = lax.top_k(jnp.swapaxes(aff, 1, 2), cap)
    xs = jax.vmap(lambda hb, ib: hb[ib])(h, idx)
    hid = jax.nn.silu(jnp.einsum('becd,edf->becf', xs, wg)) * jnp.einsum('becd,edf->becf', xs, wu)
    y = jnp.einsum('becf,efd->becd', hid, wd) * gate[..., None].astype(h.dtype)
    return jax.vmap(lambda yb, ib: jnp.zeros((n, D), h.dtype).at[ib.reshape(-1)].add(yb.reshape(-1, D)))(y, idx)


def setup_inputs(seed: int = 0) -> dict:
    key = jax.random.key(seed)
    ks = iter(jax.random.split(key, 48))

    def nrm(shape, scale):
        return jax.random.normal(next(ks), shape, F32) * scale

    D, C, H, N = D_MODEL, RW_WIDTH, RW_HEADS, RW_HEAD_DIM
    G, S, E, Fe = S5_GROUPS, S5_STATE, N_EXPERTS, EXPERT_FF
    x = nrm((BATCH, SEQ, D), 1.0)
    c = nrm((BATCH, D), 1.0)
    ctx = nrm((BATCH, CTX_LEN, D), 1.0)
    c_ctx = nrm((D,), 1.0)
    mod_w = nrm((DEPTH, D, N_MOD * D), 0.5 * D ** -0.5)
    mod_b = nrm((DEPTH, N_MOD * D), 0.02)
    norm1_g = 1.0 + nrm((DEPTH, D), 0.02)
    norm2_g = 1.0 + nrm((DEPTH, D), 0.02)
    w_in = nrm((DEPTH, D, N_IN), D ** -0.5)
    rwkv_conv = jnp.array([0.25, 0.5, 0.25], F32)[None, :, None] + nrm((DEPTH, 3, 3 * C), 0.05)
    ch = (jnp.arange(C) % N).astype(F32) / (N - 1)
    rwkv_w0 = (-6.0 + 5.0 * ch) + nrm((DEPTH, 2, C), 0.1)
    rwkv_w2 = nrm((DEPTH, 2, RW_DECAY_RANK, C), 0.1 * RW_DECAY_RANK ** -0.5)
    rwkv_a0 = nrm((DEPTH, 2, C), 0.1)
    rwkv_a2 = nrm((DEPTH, 2, RW_AAA_RANK, C), 0.1 * RW_AAA_RANK ** -0.5)
    rwkv_g2 = nrm((DEPTH, RW_GATE_RANK, C), RW_GATE_RANK ** -0.5)
    rwkv_kk = 0.85 + nrm((DEPTH, C), 0.02)
    rwkv_ka = 1.0 + nrm((DEPTH, C), 0.02)
    rwkv_rk = nrm((DEPTH, H, N), 0.1)
    rwkv_ln_g = 1.0 + nrm((DEPTH, C), 0.02)
    rwkv_ln_b = nrm((DEPTH, C), 0.02)
    s5_lam_re = -0.5 + nrm((DEPTH, 2, G, S), 0.01)
    s5_lam_im = math.pi * jnp.arange(S, dtype=F32) + nrm((DEPTH, 2, G, S), 0.01)
    s5_log_dt = math.log(S5_DT_MIN) + (math.log(S5_DT_MAX) - math.log(S5_DT_MIN)) * jax.random.uniform(next(ks), (DEPTH, 2, G), F32)
    s5_b_re = nrm((DEPTH, 2, G, S, S5_GROUP), (2 * S5_GROUP) ** -0.5)
    s5_b_im = nrm((DEPTH, 2, G, S, S5_GROUP), (2 * S5_GROUP) ** -0.5)
    s5_c_re = nrm((DEPTH, 2, G, S5_GROUP, S), (2 * S) ** -0.5)
    s5_c_im = nrm((DEPTH, 2, G, S5_GROUP, S), (2 * S) ** -0.5)
    s5_d = nrm((DEPTH, S5_WIDTH), 1.0)
    s5_glu = nrm((DEPTH, S5_WIDTH, S5_WIDTH), S5_WIDTH ** -0.5)
    attn_qn = 1.0 + nrm((DEPTH, AT_HEAD_DIM), 0.02)
    attn_kn = 1.0 + nrm((DEPTH, AT_HEAD_DIM), 0.02)
    w_branch = jnp.concatenate([nrm((DEPTH, RW_WIDTH, D), RW_WIDTH ** -0.5),
                                nrm((DEPTH, S5_WIDTH, D), S5_WIDTH ** -0.5),
                                nrm((DEPTH, AT_WIDTH, D), AT_WIDTH ** -0.5)], axis=1)
    w_out = nrm((DEPTH, D, D), D ** -0.5)
    router = nrm((DEPTH, D, E), D ** -0.5)
    exp_gate = nrm((DEPTH, E, D, Fe), D ** -0.5)
    exp_up = nrm((DEPTH, E, D, Fe), D ** -0.5)
    exp_down = nrm((DEPTH, E, Fe, D), Fe ** -0.5)
    final_g = 1.0 + nrm((D,), 0.02)
    return {"x": x, "c": c, "ctx": ctx, "c_ctx": c_ctx, "mod_w": mod_w, "mod_b": mod_b,
            "norm1_g": norm1_g, "norm2_g": norm2_g, "w_in": w_in, "rwkv_conv": rwkv_conv,
            "rwkv_w0": rwkv_w0, "rwkv_w2": rwkv_w2, "rwkv_a0": rwkv_a0, "rwkv_a2": rwkv_a2,
            "rwkv_g2": rwkv_g2, "rwkv_kk": rwkv_kk, "rwkv_ka": rwkv_ka, "rwkv_rk": rwkv_rk,
            "rwkv_ln_g": rwkv_ln_g, "rwkv_ln_b": rwkv_ln_b, "s5_lam_re": s5_lam_re,
            "s5_lam_im": s5_lam_im, "s5_log_dt": s5_log_dt, "s5_b_re": s5_b_re, "s5_b_im": s5_b_im,
            "s5_c_re": s5_c_re, "s5_c_im": s5_c_im, "s5_d": s5_d, "s5_glu": s5_glu,
            "attn_qn": attn_qn, "attn_kn": attn_kn, "w_branch": w_branch, "w_out": w_out,
            "router": router, "exp_gate": exp_gate, "exp_up": exp_up, "exp_down": exp_down,
            "final_g": final_g}


def reference(x, c, ctx, c_ctx, mod_w, mod_b, norm1_g, norm2_g, w_in, rwkv_conv,
              rwkv_w0, rwkv_w2, rwkv_a0, rwkv_a2, rwkv_g2, rwkv_kk, rwkv_ka, rwkv_rk,
              rwkv_ln_g, rwkv_ln_b, s5_lam_re, s5_lam_im, s5_log_dt, s5_b_re, s5_b_im,
              s5_c_re, s5_c_im, s5_d, s5_glu, attn_qn, attn_kn, w_branch, w_out,
              router, exp_gate, exp_up, exp_down, final_g):
    B, L, D = x.shape
    rows = L // GRID_W
    row = jnp.repeat(jnp.arange(rows, dtype=F32), GRID_W)
    col = jnp.tile(jnp.arange(GRID_W, dtype=F32), rows)
    freqs = ROPE_THETA ** (-jnp.arange(ROPE_AXIS_DIM // 2, dtype=F32) / (ROPE_AXIS_DIM // 2))
    ang_row = row[:, None] * freqs
    ang_col = col[:, None] * freqs

    sc = jax.nn.silu(c)
    scc = jax.nn.silu(c_ctx)
    xl, xc = x, ctx
    for l in range(DEPTH):
        need_ctx = l < DEPTH - 1
        mod_l = (sc @ mod_w[l] + mod_b[l]).reshape(B, 1, N_MOD, D)
        mod_c = (scc @ mod_w[l] + mod_b[l]).reshape(1, 1, N_MOD, D)
        hl = modulate(rms_norm(xl, norm1_g[l]), mod_l[:, :, 0], mod_l[:, :, 1])
        hc = modulate(rms_norm(xc, norm1_g[l]), mod_c[:, :, 0], mod_c[:, :, 1])
        pl = hl @ w_in[l]
        pc = hc @ (w_in[l] if need_ctx else w_in[l][:, :OFF_GATE])
        rw_c, rw_l = rwkv_mix(pc, pl, rwkv_conv[l], rwkv_w0[l], rwkv_w2[l], rwkv_a0[l], rwkv_a2[l],
                              rwkv_g2[l], rwkv_kk[l], rwkv_ka[l], rwkv_rk[l], rwkv_ln_g[l],
                              rwkv_ln_b[l], need_ctx)
        s5_c, s5_l = s5_mix(pc, pl, s5_lam_re[l], s5_lam_im[l], s5_log_dt[l], s5_b_re[l], s5_b_im[l],
                            s5_c_re[l], s5_c_im[l], s5_d[l], s5_glu[l], need_ctx)
        at_c, at_l = attn_mix(pc, pl, attn_qn[l], attn_kn[l], ang_row, ang_col, need_ctx)
        xl = xl + mod_l[:, :, 2] * merge(pl, rw_l, s5_l, at_l, w_branch[l], w_out[l])
        hl2 = modulate(rms_norm(xl, norm2_g[l]), mod_l[:, :, 3], mod_l[:, :, 4])
        xl = xl + mod_l[:, :, 5] * ec_moe(hl2, router[l], exp_gate[l], exp_up[l], exp_down[l])
        if need_ctx:
            xc = xc + mod_c[:, :, 2] * merge(pc, rw_c, s5_c, at_c, w_branch[l], w_out[l])
            hc2 = modulate(rms_norm(xc, norm2_g[l]), mod_c[:, :, 3], mod_c[:, :, 4])
            xc = xc + mod_c[:, :, 5] * ec_moe(hc2, router[l], exp_gate[l], exp_up[l], exp_down[l])
    return rms_norm(xl, final_g)
```

```python
import contextlib
import math
import numpy as np
import concourse.bass as bass
import concourse.mybir as mybir
from concourse.bass_utils import run_bass_kernel_spmd

F32 = mybir.dt.float32
BF16 = mybir.dt.bfloat16
I32 = mybir.dt.int32
AF = mybir.ActivationFunctionType
ALU = mybir.AluOpType
AX = mybir.AxisListType

_DTSZ = {F32: 4, BF16: 2, I32: 4}


def dtsize(dt):
    return _DTSZ[dt]


def ap_box(ap):
    es = dtsize(ap.dtype)
    off = int(ap.offset) * es
    dims = [(int(s) * es, int(c)) for (s, c) in ap.ap if int(c) > 1 and int(s) != 0]
    name = ap.tensor.name
    if not dims:
        return (name, 0, 0, 0, off, off + es, off, off + es)
    S = max(abs(s) for s, _ in dims)
    lo = off + sum(min(0, s * (c - 1)) for s, c in dims)
    hi = off + sum(max(0, s * (c - 1)) for s, c in dims) + es
    big = [(s, c) for s, c in dims if abs(s) == S]
    rest = [(s, c) for s, c in dims if abs(s) != S]
    if len(big) == 1 and big[0][0] > 0:
        r_lo = off // S
        r_hi = r_lo + big[0][1] - 1
        c_lo = off % S + sum(min(0, s * (c - 1)) for s, c in rest)
        c_hi = off % S + sum(max(0, s * (c - 1)) for s, c in rest) + es
        if c_lo >= 0 and c_hi <= S:
            return (name, S, r_lo, r_hi, c_lo, c_hi, lo, hi)
    return (name, 0, 0, 0, lo, hi, lo, hi)


def boxes_overlap(a, b):
    if a[1] == b[1] and a[1] != 0:
        return not (a[3] < b[2] or b[3] < a[2] or a[5] <= b[4] or b[5] <= a[4])
    return not (a[7] <= b[6] or b[7] <= a[6])


def box_contains(a, b):
    if a[1] == b[1] and a[1] != 0:
        return a[2] <= b[2] and a[3] >= b[3] and a[4] <= b[4] and a[5] >= b[5]
    if a[1] == 0 and b[1] == 0:
        return a[6] <= b[6] and a[7] >= b[7]
    return False


class Op:
    __slots__ = ("eng", "fn", "idx", "waits", "signal", "tick", "is_dma", "dsem", "dval")

    def __init__(self, eng, fn, is_dma):
        self.eng = eng
        self.fn = fn
        self.is_dma = is_dma
        self.waits = {}
        self.signal = False
        self.tick = 0
        self.dsem = None
        self.dval = 0


ENGS = ("pe", "act", "dve", "pool", "sp")
N_DMA_SEMS = 32


class Sched:
    def __init__(self, nc):
        self.nc = nc
        self.ops = []
        self.by_eng = {e: [] for e in ENGS}
        self.tens = {}
        self.n_dma = 0
        self.dma_last = {}
        self.barrier_snap = None
        self.bar_done = set()

    def barrier(self):
        lasts = []
        for e in ENGS:
            for op in reversed(self.by_eng[e]):
                if not op.is_dma and op.fn is not None:
                    lasts.append(op)
                    break
        self.barrier_snap = (lasts, {k: p.dval for k, p in self.dma_last.items()})
        self.bar_done = set()

    def _need(self, op, prod):
        if prod is op:
            return
        if prod.is_dma:
            key = ("d", prod.dsem)
            val = prod.dval
            cur = op.waits.get(key)
            op.waits[key] = val if cur is None else max(cur, val)
        else:
            if prod.eng == op.eng and op.eng == "pe" and not op.is_dma:
                return
            key = ("e", prod.eng)
            prod.signal = True
            cur = op.waits.get(key)
            if cur is None or prod.idx > cur.idx:
                op.waits[key] = prod

    def add(self, eng, fn, reads=(), writes=(), is_dma=False):
        op = Op(eng, fn, is_dma)
        op.idx = len(self.ops)
        if is_dma:
            k = self.n_dma % N_DMA_SEMS
            op.dsem = k
            op.dval = 16 * (self.n_dma // N_DMA_SEMS + 1)
            prev = self.dma_last.get(k)
            if prev is not None:
                self._need(op, prev)
            self.dma_last[k] = op
            self.n_dma += 1
        rb = [ap_box(a) for a in reads]
        wb = [ap_box(a) for a in writes]
        if self.barrier_snap is not None:
            for bx in rb + wb:
                if bx[0] not in self.tens and bx[0] not in self.bar_done:
                    self.bar_done.add(bx[0])
                    for p in self.barrier_snap[0]:
                        self._need(op, p)
                    for k, v in self.barrier_snap[1].items():
                        key = ("d", k)
                        cur = op.waits.get(key)
                        op.waits[key] = v if cur is None else max(cur, v)
        for bx in rb:
            for r in self.tens.setdefault(bx[0], []):
                if r[1] == "w" and boxes_overlap(r[0], bx):
                    self._need(op, r[2])
        for bx in wb:
            for r in self.tens.setdefault(bx[0], []):
                if boxes_overlap(r[0], bx):
                    if r[1] == "r" and r[2].eng == eng and not r[2].is_dma and not is_dma:
                        continue
                    self._need(op, r[2])
        for bx in rb:
            recs = self.tens[bx[0]]
            recs[:] = [r for r in recs if not (r[1] == "r" and r[2].eng == eng and (not r[2].is_dma) and (not is_dma)
                                               and box_contains(bx, r[0]))]
            recs.append((bx, "r", op))
        for bx in wb:
            recs = self.tens[bx[0]]
            recs[:] = [r for r in recs if not box_contains(bx, r[0])]
            recs.append((bx, "w", op))
        self.ops.append(op)
        self.by_eng[eng].append(op)
        return op

    def finish(self, eng="sp"):
        op = Op(eng, None, False)
        op.idx = len(self.ops)
        for k, p in self.dma_last.items():
            op.waits[("d", k)] = p.dval
        self.ops.append(op)
        self.by_eng[eng].append(op)

    def emit(self):
        nc = self.nc
        for e in ENGS:
            t = 0
            for op in self.by_eng[e]:
                if op.signal and not op.is_dma:
                    t += 1
                    op.tick = t
        with contextlib.ExitStack() as st:
            esem = {e: st.enter_context(nc.semaphore("se_" + e)) for e in ENGS}
            dsem = [st.enter_context(nc.semaphore("sd_%d" % i)) for i in range(N_DMA_SEMS)]
            block = st.enter_context(nc.Block())
            ops_by = self.by_eng

            def run(engname, e):
                known = {}
                for op in ops_by[engname]:
                    for key, val in op.waits.items():
                        if key[0] == "e":
                            v = val.tick
                            sem = esem[key[1]]
                        else:
                            v = val
                            sem = dsem[key[1]]
                        if known.get(key, 0) >= v:
                            continue
                        known[key] = v
                        e.wait_ge(sem, v)
                    if op.fn is None:
                        continue
                    ins = op.fn(e)
                    if op.is_dma:
                        ins.then_inc(dsem[op.dsem], 16)
                    elif op.signal:
                        ins.then_inc(esem[engname], 1)

            @block.tensor
            def _(e):
                run("pe", e)

            @block.scalar
            def _(e):
                run("act", e)

            @block.vector
            def _(e):
                run("dve", e)

            @block.gpsimd
            def _(e):
                run("pool", e)

            @block.sync
            def _(e):
                run("sp", e)


D = 2048
DEPTH = 4
N_MOD = 6
EPS = 1e-6
RW_H, RW_N, RW_W = 8, 64, 512
RW_LN_EPS = 64e-5
S5_W, S5_GRP, S5_G, S5_ST = 512, 16, 32, 64
AT_H, AT_KV, AT_D, AT_W, AT_KVW = 8, 2, 128, 1024, 256
OFF_WD = 1536
OFF_AD = OFF_WD + 128
OFF_GD = OFF_AD + 128
OFF_S5 = OFF_GD + 128
OFF_Q = OFF_S5 + 512
OFF_K = OFF_Q + 1024
OFF_V = OFF_K + 256
OFF_GATE = OFF_V + 256
N_IN = OFF_GATE + 3 * D
NTOKC = N_IN - OFF_Q
NE, FF = 16, 1024
GRID_W = 64
G = 4
GROUPS4 = [[0, 1, 2, 3], [4, 5, 6, 7]]
GROUP8 = [list(range(8))]


class Cfg:
    def __init__(self, lt=16, depth=DEPTH, stop_after=None, debug=()):
        self.LT = lt
        self.NT = lt + 1
        self.TOK = self.NT * 128
        self.depth = depth
        self.L = 4 * lt * 128
        self.CTX = 256
        self.stop_after = stop_after
        self.debug = tuple(debug)


class B:
    def __init__(self, cfg):
        self.cfg = cfg
        self.nc = bass.Bass("TRN2", target_bir_lowering=False)
        self.S = Sched(self.nc)
        self.st = contextlib.ExitStack()
        self.ins = {}
        self.outs = {}
        self._n = 0

    def inp(self, name, shape, dt=F32):
        t = self.nc.dram_tensor(name, list(shape), dt, kind="ExternalInput").ap()
        self.ins[name] = (tuple(shape), dt)
        return t

    def out(self, name, shape, dt=F32):
        t = self.nc.dram_tensor(name, list(shape), dt, kind="ExternalOutput").ap()
        self.outs[name] = (tuple(shape), dt)
        return t

    def dram(self, name, shape, dt=F32):
        return self.nc.dram_tensor(name, list(shape), dt, kind="Internal").ap()

    def sb(self, name, shape, dt=F32):
        self._n += 1
        return self.st.enter_context(self.nc.sbuf_tensor("%s_%d" % (name, self._n), list(shape), dt))

    def ps(self, name, shape, dt=F32):
        self._n += 1
        return self.st.enter_context(self.nc.psum_tensor("%s_%d" % (name, self._n), list(shape), dt))

    def dma(self, out, in_, eng="sp"):
        return self.S.add(eng, lambda e: e.dma_start(out=out, in_=in_), reads=[in_], writes=[out], is_dma=True)

    def coll(self, kind, op, groups, in_, out):
        return self.S.add("pool", lambda e: e.collective_compute(kind, op, replica_groups=groups, ins=[in_], outs=[out]),
                          reads=[in_], writes=[out], is_dma=True)

    def ag_in(self, src, dst, groups=None):
        self._n += 1
        stg = self.dram("stg%d" % self._n, list(src.shape), src.dtype)
        self.dma(stg, src, eng="act")
        return self.coll("AllGather", ALU.bypass, groups or GROUP8, stg, dst)

    def mm(self, out, lhsT, rhs, start=True, stop=True):
        return self.S.add("pe", lambda e: e.matmul(out, lhsT, rhs, start=start, stop=stop), reads=[lhsT, rhs], writes=[out])

    def tr(self, out, in_, ident):
        return self.S.add("pe", lambda e: e.transpose(out, in_, ident), reads=[in_, ident], writes=[out])

    def act(self, out, in_, func, bias=None, scale=None, accum_out=None, eng="act"):
        kw = {}
        rd = [in_]
        wr = [out]
        if bias is not None:
            kw["bias"] = bias
            if not isinstance(bias, (int, float)):
                rd.append(bias)
        if scale is not None:
            kw["scale"] = scale
            if not isinstance(scale, (int, float)):
                rd.append(scale)
        if accum_out is not None:
            kw["accum_out"] = accum_out
            wr.append(accum_out)
        return self.S.add(eng, lambda e: e.activation(out=out, in_=in_, func=func, **kw), reads=rd, writes=wr)

    def copy(self, out, in_, eng="dve"):
        if eng == "act":
            return self.S.add("act", lambda e: e.copy(out, in_), reads=[in_], writes=[out])
        return self.S.add(eng, lambda e: e.tensor_copy(out, in_), reads=[in_], writes=[out])

    def tt(self, out, in0, in1, op, eng="dve"):
        return self.S.add(eng, lambda e: e.tensor_tensor(out, in0, in1, op), reads=[in0, in1], writes=[out])

    def ts(self, out, in0, s1, s2=None, op0=ALU.mult, op1=None, eng="dve", accum_out=None):
        rd = [in0] + [s for s in (s1, s2) if s is not None and not isinstance(s, (int, float))]
        wr = [out] + ([accum_out] if accum_out is not None else [])
        if op1 is None:
            return self.S.add(eng, lambda e: e.tensor_single_scalar(out, in0, s1, op0), reads=rd, writes=wr)
        kw = {"accum_out": accum_out} if accum_out is not None else {}
        return self.S.add(eng, lambda e: e.tensor_scalar(out, in0, s1, s2, op0, op1, **kw), reads=rd, writes=wr)

    def stt(self, out, in0, scalar, in1, op0, op1, eng="dve"):
        rd = [in0, in1] + ([scalar] if not isinstance(scalar, (int, float)) else [])
        return self.S.add(eng, lambda e: e.scalar_tensor_tensor(out, in0, scalar, in1, op0, op1), reads=rd, writes=[out])

    def memset(self, out, val, eng="pool"):
        return self.S.add(eng, lambda e: e.memset(out, val), writes=[out])

    def reduce(self, out, in_, op=ALU.add, axis=AX.X, eng="dve"):
        return self.S.add(eng, lambda e: e.tensor_reduce(out, in_, axis, op), reads=[in_], writes=[out])

    def scan(self, out, d0, d1, init, op0=ALU.mult, op1=ALU.add, eng="dve"):
        rd = [d0, d1] + ([init] if not isinstance(init, (int, float)) else [])
        return self.S.add(eng, lambda e: e.tensor_tensor_scan(out, d0, d1, init, op0, op1), reads=rd, writes=[out])

    def rstd(self, out, in_, scale, eps):
        self.ts(out, in_, scale, eps, ALU.mult, ALU.add)
        self.act(out, out, AF.Sqrt)
        return self.recip(out, out)

    def recip(self, out, in_):
        return self.S.add("dve", lambda e: e.reciprocal(out, in_), reads=[in_], writes=[out])


def bcast_row(ap_row, n=128):
    return ap_row.broadcast_to([n, ap_row.shape[-1]])


CT = 2


class Cfg:
    def __init__(self, nlt=64, depth=DEPTH, stop_after=None, debug=()):
        self.NLT = nlt
        self.NT = nlt + CT
        self.TOK = self.NT * 128
        self.depth = depth
        self.L = nlt * 128
        self.stop_after = stop_after
        self.debug = tuple(debug)
        self.groups = [[0, 1]] + [list(range(CT + i, min(CT + i + 8, self.NT))) for i in range(0, nlt, 8)]


SEGS = ([(0, 512), (512, 512), (1024, 512), (1536, 384), (1920, 512), (2432, 512), (2944, 512), (3456, 512)]
        + [(OFF_GATE + i * 512, 512) for i in range(12)])


def build(cfg):
    b = B(cfg)
    nc, S = b.nc, b.S
    NT, TOK, dl = cfg.NT, cfg.TOK, cfg.depth
    KC = D // 128

    def debug_out(name, src_ap, shape, dt=F32):
        if name in cfg.debug:
            o = b.out("dbg_" + name, shape, dt)
            b.dma(o, src_ap)

    x_in = b.inp("x_seq", [TOK, D])
    csT = b.inp("csT", [128, KC, 2])
    mod_w = b.inp("mod_w", [dl, D, N_MOD * D])
    modb = b.inp("modb", [dl, 1, N_MOD * D])
    norm1_g = b.inp("norm1_g", [dl, 1, D])
    norm2_g = b.inp("norm2_g", [dl, 1, D])
    final_g = b.inp("final_g", [1, D])
    w_in = b.inp("w_in", [dl, D, N_IN])
    ropeCS = b.inp("ropeCS", [TOK, 128])
    ropeSN = b.inp("ropeSN", [TOK, 128])
    attn_qn = b.inp("attn_qn", [dl, 1, 128])
    attn_kn = b.inp("attn_kn", [dl, 1, 128])
    ident_in = b.inp("ident", [128, 128])
    masks_in = b.inp("masks", [6, 128, 128])
    rw_conv = b.inp("rw_conv", [dl, 3, 1, 1536])
    rw_w0 = b.inp("rw_w0", [dl, 1, 1024])
    rw_a0 = b.inp("rw_a0", [dl, 1, 1024])
    rw_w2 = b.inp("rw_w2", [dl, 128, 512])
    rw_a2 = b.inp("rw_a2", [dl, 128, 512])
    rw_g2 = b.inp("rw_g2", [dl, 128, 512])
    rw_kk = b.inp("rw_kk", [dl, 1, 512])
    rw_ka = b.inp("rw_ka", [dl, 1, 512])
    rw_rk = b.inp("rw_rk", [dl, 1, 512])
    rw_lng = b.inp("rw_lng", [dl, 1, 512])
    rw_lnb = b.inp("rw_lnb", [dl, 1, 512])
    s5_par = b.inp("s5_par", [dl, 128, 3, 2, 16])
    s5_bmat = b.inp("s5_bmat", [dl, 128, 2, 2, 16, 32])
    s5_cmat = b.inp("s5_cmat", [dl, 128, 2, 2, 16, 32])
    s5_glu = b.inp("s5_glu", [dl, 512, 512])
    s5_dskip = b.inp("s5_dskip", [dl, 32, 16])
    iota_in = b.inp("iota512", [1, 512])
    w_branch = b.inp("w_branch", [dl, D, D])
    w_out = b.inp("w_out", [dl, D, D])
    router = b.inp("router", [dl, D, NE])
    exp_gate = b.inp("exp_gate", [dl, NE, D, FF])
    exp_up = b.inp("exp_up", [dl, NE, D, FF])
    exp_down = b.inp("exp_down", [dl, NE, FF, D])

    pst = b.st
    ident_f = b.sb("ident_f", [128, 128])
    ident_b = b.sb("ident_b", [128, 128], BF16)
    b.dma(ident_f[:], ident_in)
    b.copy(ident_b[:], ident_f[:])

    Xcur = b.dram("Xcur", [TOK, D])
    modsel = b.dram("modsel", [dl, 2, N_MOD * D])
    Prw = b.dram("Prw", [TOK, 1920])
    Ps5 = b.dram("Ps5", [TOK, 512])
    QTd = b.dram("QTd", [AT_H, 128, TOK], BF16)
    KTd = b.dram("KTd", [AT_KV, 128, TOK], BF16)
    Vd = b.dram("Vd", [TOK, AT_KVW], BF16)
    Gd = b.dram("Gd", [TOK, 3 * D])
    Orw = b.dram("Orw", [TOK, 512])
    Yrw = b.dram("Yrw", [TOK, 512], BF16)
    Ys5T = b.dram("Ys5T", [32, 16, TOK])
    OTd = b.dram("OTd", [AT_H, 128, TOK], BF16)
    MTd = b.dram("MTd", [KC, 128, TOK], BF16)
    H2Td = b.dram("H2Td", [KC, 128, TOK], BF16)
    AFFd = b.dram("AFFd", [TOK, NE])
    ACCd = b.dram("ACCd", [TOK, D])
    Ys5 = b.dram("Ys5", [TOK, 512], BF16)

    def scope():
        st = contextlib.ExitStack()
        b.st = st
        return st

    def end_scope(st):
        st.close()
        S.barrier()
        b.st = pst

    st0 = scope()
    cs = b.sb("cs", [128, KC, 2])
    b.dma(cs[:], csT)
    b.act(cs[:], cs[:], AF.Silu)
    mwb = [b.sb("mwb%d" % i, [128, KC, 512]) for i in range(2)]
    mb2 = b.sb("mb2", [2, N_MOD * D])
    msel = b.sb("msel", [2, N_MOD * D])
    pm = [b.ps("pm%d" % i, [2, 512]) for i in range(2)]
    for l in range(dl):
        b.dma(mb2[:], modb[l].broadcast_to([2, N_MOD * D]))
        for n in range(N_MOD * D // 512):
            w_ = mwb[n % 2]
            b.dma(w_[:], mod_w[l][:, n * 512:(n + 1) * 512].rearrange("(k p) n -> p k n", p=128), eng="sp" if n % 2 else "act")
            p_ = pm[n % 2]
            for k in range(KC):
                b.mm(p_[:], cs[:, k, :], w_[:, k, :], start=(k == 0), stop=(k == KC - 1))
            b.tt(msel[:, n * 512:(n + 1) * 512], p_[:], mb2[:, n * 512:(n + 1) * 512], ALU.add)
        b.dma(modsel[l], msel[:])
    end_scope(st0)
    debug_out("modsel", modsel.rearrange("l r n -> (l r) n"), [dl * 2, N_MOD * D])

    def modrow(l, which, m):
        return modsel[l, which:which + 1, m * D:(m + 1) * D]

    b.dma(Xcur, x_in)

    for l in range(dl):
        st1 = scope()
        G1 = b.sb("G1", [128, 2, D])
        SH = b.sb("SH", [128, 2, D])
        gt = b.sb("gt", [128, D])
        b.dma(gt[:], norm1_g[l].broadcast_to([128, D]))
        for w in range(2):
            b.dma(G1[:, w, :], bcast_row(modrow(l, w, 1)))
            b.dma(SH[:, w, :], bcast_row(modrow(l, w, 0)))
            b.stt(G1[:, w, :], G1[:, w, :], 1.0, gt[:], ALU.add, ALU.mult)
        hT = b.sb("hT", [128, KC, 1024], BF16)
        xt = [b.sb("xtA%d" % i, [128, D]) for i in range(2)]
        hb = [b.sb("hbA%d" % i, [128, D], BF16) for i in range(2)]
        ss = b.sb("ssA", [128, 2])
        pT = [b.ps("pTA%d" % i, [128, 8, 128], BF16) for i in range(2)]
        wblk = [b.sb("wblk%d" % i, [128, KC, 512], BF16) for i in range(2)]
        pp = [b.ps("ppB%d" % i, [128, 512]) for i in range(2)]
        pTq = [b.ps("pTq%d" % i, [128, 4, 128], BF16) for i in range(2)]
        qraw = b.sb("qraw", [128, 512])
        qtmp = b.sb("qtmp", [128, 512])
        qbf = b.sb("qbf", [128, 512], BF16)
        nrm = b.sb("nrmB", [128, 4])
        qn_t = b.sb("qn_t", [128, 128])
        kn_t = b.sb("kn_t", [128, 128])
        b.dma(qn_t[:], attn_qn[l].broadcast_to([128, 128]))
        b.dma(kn_t[:], attn_kn[l].broadcast_to([128, 128]))
        cs_t = b.sb("cs_t", [128, 8, 128])
        sn_t = b.sb("sn_t", [128, 8, 128])
        osb = [b.sb("osb%d" % i, [128, 512]) for i in range(2)]
        qTs = b.sb("qTs", [128, 4, 1024], BF16)
        kTs = b.sb("kTs", [128, 2, 1024], BF16)
        vbf = b.sb("vbf", [128, 8, AT_KVW], BF16)

        def norm_rope(nheads, gain_t, j):
            W = nheads * 128
            b.act(qtmp[:, :W], qraw[:, :W], AF.Square)
            b.reduce(nrm[:, :nheads], qtmp[:, :W].rearrange("p (h d) -> p h d", d=128))
            b.rstd(nrm[:, :nheads], nrm[:, :nheads], 1.0 / 128, EPS)
            q3 = qraw[:, :W].rearrange("p (h d) -> p h d", d=128)
            b.tt(q3, q3, nrm[:, :nheads].unsqueeze(2).broadcast_to([128, nheads, 128]), ALU.mult)
            b.tt(q3, q3, gain_t[:].unsqueeze(1).broadcast_to([128, nheads, 128]), ALU.mult)
            x5 = qraw[:, :W].rearrange("p (h a f c) -> p h a f c", a=2, f=2, c=32)
            t5 = qtmp[:, :W].rearrange("p (h a f c) -> p h a f c", a=2, f=2, c=32)
            sn4 = sn_t[:, j, :].rearrange("p (a f c) -> p a f c", a=2, f=2)
            cs3 = cs_t[:, j, :].unsqueeze(1).broadcast_to([128, nheads, 128])
            for f in range(2):
                b.tt(t5[:, :, :, f, :], x5[:, :, :, 1 - f, :],
                     sn4[:, :, f, :].unsqueeze(1).broadcast_to([128, nheads, 2, 32]), ALU.mult, eng="pool")
            b.tt(q3, q3, cs3, ALU.mult)
            b.tt(qbf[:, :W], qraw[:, :W], qtmp[:, :W], ALU.add)

        for grp in cfg.groups:
            ng = len(grp)
            t0 = grp[0]
            r0, r1 = t0 * 128, (t0 + ng) * 128
            w = 1 if t0 < CT else 0
            b.dma(cs_t[:, :ng, :], ropeCS[r0:r1, :].rearrange("(t p) c -> p t c", p=128))
            b.dma(sn_t[:, :ng, :], ropeSN[r0:r1, :].rearrange("(t p) c -> p t c", p=128))
            for j, t in enumerate(grp):
                x_ = xt[j % 2]
                h_ = hb[j % 2]
                sc = ss[:, j % 2:j % 2 + 1]
                b.dma(x_[:], Xcur[t * 128:(t + 1) * 128, :], eng="sp" if j % 2 else "act")
                b.memset(sc, 0.0, eng="dve")
                b.act(h_[:], x_[:], AF.Square, accum_out=sc)
                b.rstd(sc, sc, 1.0 / D, EPS)
                b.stt(x_[:], x_[:], sc, G1[:, w, :], ALU.mult, ALU.mult)
                b.tt(h_[:], x_[:], SH[:, w, :], ALU.add, eng="pool")
                for half in range(2):
                    p_ = pT[half]
                    for k in range(8):
                        kc = half * 8 + k
                        b.tr(p_[:, k, :], h_[:, kc * 128:(kc + 1) * 128], ident_b[:])
                    b.copy(hT[:, half * 8:(half + 1) * 8, j * 128:(j + 1) * 128], p_[:], eng="act" if half else "dve")
            for si, (c0, cw) in enumerate(SEGS):
                w_ = wblk[si % 2]
                b.dma(w_[:, :, :cw], w_in[l][:, c0:c0 + cw].rearrange("(k p) n -> p k n", p=128), eng="pool")
                for j, t in enumerate(grp):
                    p_ = pp[j % 2]
                    for k in range(KC):
                        b.mm(p_[:, :cw], hT[:, k, j * 128:(j + 1) * 128], w_[:, k, :cw], start=(k == 0), stop=(k == KC - 1))
                    rows = slice(t * 128, (t + 1) * 128)
                    if si <= 3:
                        o_ = osb[j % 2]
                        b.copy(o_[:, :cw], p_[:, :cw], eng="act" if j % 2 else "dve")
                        b.dma(Prw[rows, c0:c0 + cw], o_[:, :cw])
                    elif si == 4:
                        o_ = osb[j % 2]
                        b.copy(o_[:], p_[:], eng="act" if j % 2 else "dve")
                        b.dma(Ps5[rows, :], o_[:])
                    elif si in (5, 6):
                        b.copy(qraw[:], p_[:], eng="act")
                        norm_rope(4, qn_t, j)
                        pq = pTq[j % 2]
                        for hh in range(4):
                            b.tr(pq[:, hh, :], qbf[:, hh * 128:(hh + 1) * 128], ident_b[:])
                        b.copy(qTs[:, :, j * 128:(j + 1) * 128], pq[:], eng="act")
                    elif si == 7:
                        b.copy(qraw[:], p_[:], eng="act")
                        b.copy(vbf[:, j, :], qraw[:, 256:512])
                        norm_rope(2, kn_t, j)
                        pq = pTq[j % 2]
                        for hh in range(2):
                            b.tr(pq[:, hh, :], qbf[:, hh * 128:(hh + 1) * 128], ident_b[:])
                        b.copy(kTs[:, :, j * 128:(j + 1) * 128], pq[:, 0:2, :], eng="act")
                    else:
                        o_ = osb[j % 2]
                        b.act(o_[:], p_[:], AF.Sigmoid)
                        b.dma(Gd[rows, c0 - OFF_GATE:c0 - OFF_GATE + 512], o_[:])
                if si in (5, 6):
                    hq = (si - 5) * 4
                    b.dma(QTd[hq:hq + 4, :, r0:r1].rearrange("h p t -> p h t"), qTs[:, :, :ng * 128])
                if si == 7:
                    b.dma(KTd[:, :, r0:r1].rearrange("h p t -> p h t"), kTs[:, :, :ng * 128])
                    b.dma(Vd[r0:r1, :].rearrange("(t p) c -> p t c", p=128), vbf[:, :ng, :])
        end_scope(st1)
        debug_out("Prw", Prw, [TOK, 1920])
        debug_out("Ps5", Ps5, [TOK, 512])
        debug_out("QT", QTd.rearrange("h p t -> (h p) t"), [AT_H * 128, TOK], BF16)
        debug_out("KT", KTd.rearrange("h p t -> (h p) t"), [AT_KV * 128, TOK], BF16)
        debug_out("V", Vd, [TOK, AT_KVW], BF16)
        debug_out("G", Gd, [TOK, 3 * D])
        if cfg.stop_after == "P1":
            break

        st2 = scope()
        cw = b.sb("rw_cw", [128, 3, 1536])
        for i in range(3):
            b.dma(cw[:, i, :], rw_conv[l, i].broadcast_to([128, 1536]))
        w0t = b.sb("rw_w0t", [128, 1024])
        a0t = b.sb("rw_a0t", [128, 1024])
        b.dma(w0t[:], rw_w0[l].broadcast_to([128, 1024]))
        b.dma(a0t[:], rw_a0[l].broadcast_to([128, 1024]))
        w2t = b.sb("rw_w2t", [128, 512])
        a2t = b.sb("rw_a2t", [128, 512])
        g2t = b.sb("rw_g2t", [128, 512])
        b.dma(w2t[:], rw_w2[l])
        b.dma(a2t[:], rw_a2[l])
        b.dma(g2t[:], rw_g2[l])
        vecs = b.sb("rw_vecs", [128, 5, 512])
        for i, src in enumerate((rw_kk, rw_ka, rw_rk, rw_lng, rw_lnb)):
            b.dma(vecs[:, i, :], src[l].broadcast_to([128, 512]))
        mk = b.sb("rw_mk", [128, 6, 128])
        b.dma(mk[:], masks_in.rearrange("m s t -> s m t"))
        ones_t = b.sb("rw_ones", [128, 128])
        b.memset(ones_t[:], 1.0)
        i64r = b.sb("rw_i64r", [64, 8, 64])
        b.copy(i64r[:], ident_f[0:64, 0:64].unsqueeze(1).broadcast_to([64, 8, 64]), eng="pool")
        i128r = ident_f[:].unsqueeze(1).broadcast_to([128, 8, 128])

        p3 = b.sb("rw_p3", [128, 3, 1536])
        wag = b.sb("rw_wag", [128, 384])
        rkv = b.sb("rw_rkv", [128, 1536])
        tmpA = b.sb("rw_tmpA", [128, 1536])
        lrT = b.sb("rw_lrT", [128, 3, 128])
        logw = b.sb("rw_logw", [128, 1024])
        aa = b.sb("rw_aa", [128, 1024])
        gg = b.sb("rw_gg", [128, 512])
        kkn = b.sb("rw_kkn", [128, 512])
        kd = b.sb("rw_kd", [128, 2, 512])
        kka = b.sb("rw_kka", [128, 512])
        sm8 = b.sb("rw_sm8", [128, 4, 8])
        ex = b.sb("rw_ex", [128, 5, 512])
        tots = b.sb("rw_tots", [128, 512])
        tmk = b.sb("rw_tmk", [128, 6, 512])
        fT = b.sb("rw_fT", [64, 4, 8, 128])
        A_ = {n: b.sb("rw_" + n, [128, 8, 128]) for n in ("F", "G", "Fp", "R0", "R1", "nAkk", "Ark", "Arb")}
        Zs = b.sb("rw_Zs", [128, 512])
        nU0 = b.sb("rw_nU0", [128, 512])
        Qh = b.sb("rw_Qh", [128, 512])
        Phi = b.sb("rw_Phi", [64, 512])
        dW = b.sb("rw_dW", [64, 512])
        RhT = b.sb("rw_RhT", [64, 8, 128])
        STs = [b.sb("rw_ST%d" % i, [64, 8, 64]) for i in range(2)]
        ysb = b.sb("rw_ysb", [128, 512])
        yo = b.sb("rw_yo", [128, 512])
        ybf = b.sb("rw_ybf", [128, 512], BF16)
        PA = [b.ps("rw_PA%d" % i, [128, 8, 128]) for i in range(4)]
        NEG_E = -math.exp(-0.5)

        def rw_prep(c):
            t0 = c * 128
            part_lo = 0 if c < CT else CT
            part_hi = CT - 1 if c < CT else NT - 1
            b.dma(p3[:, 1, :], Prw[t0:t0 + 128, 0:1536])
            b.dma(wag[:], Prw[t0:t0 + 128, 1536:1920], eng="act")
            if c == part_lo:
                b.memset(p3[:, 0, :], 0.0)
                b.dma(p3[1:128, 0, :], Prw[t0:t0 + 127, 0:1536])
            else:
                b.dma(p3[:, 0, :], Prw[t0 - 1:t0 + 127, 0:1536])
            if c == part_hi:
                b.memset(p3[:, 2, :], 0.0)
                b.dma(p3[0:127, 2, :], Prw[t0 + 1:t0 + 128, 0:1536], eng="act")
            else:
                b.dma(p3[:, 2, :], Prw[t0 + 1:t0 + 129, 0:1536], eng="act")
            b.tt(rkv[:], p3[:, 0, :], cw[:, 0, :], ALU.mult)
            b.tt(tmpA[:], p3[:, 1, :], cw[:, 1, :], ALU.mult, eng="pool")
            b.tt(rkv[:], rkv[:], tmpA[:], ALU.add)
            b.tt(tmpA[:], p3[:, 2, :], cw[:, 2, :], ALU.mult, eng="pool")
            b.tt(rkv[:], rkv[:], tmpA[:], ALU.add)
            b.act(wag[:, 0:128], wag[:, 0:128], AF.Tanh)
            b.act(wag[:, 256:384], wag[:, 256:384], AF.Sigmoid)
            pt = PA[0]
            for i in range(3):
                b.tr(pt[:, i, :], wag[:, i * 128:(i + 1) * 128], ident_f[:])
            b.copy(lrT[:], pt[:, 0:3, :], eng="act")
            pw = PA[1]
            pa = PA[2]
            for z in range(2):
                b.mm(pw[:, z * 4:(z + 1) * 4, :], lrT[z * 64:(z + 1) * 64, 0, :], w2t[z * 64:(z + 1) * 64, :])
                b.mm(pa[:, z * 4:(z + 1) * 4, :], lrT[z * 64:(z + 1) * 64, 1, :], a2t[z * 64:(z + 1) * 64, :])
            pg = PA[3]
            b.mm(pg[:, 0:4, :], lrT[:, 2, :], g2t[:])
            b.tt(logw[:], pw[:].rearrange("p a b -> p (a b)"), w0t[:], ALU.add)
            b.act(logw[:], logw[:], AF.Sigmoid)
            b.ts(logw[:], logw[:], NEG_E, op0=ALU.mult, eng="pool")
            b.tt(aa[:], pa[:].rearrange("p a b -> p (a b)"), a0t[:], ALU.add)
            b.act(aa[:], aa[:], AF.Sigmoid)
            b.copy(gg[:], pg[:, 0:4, :].rearrange("p a b -> p (a b)"), eng="act")
            kcol = rkv[:, 512:1024]
            b.tt(kkn[:], kcol, vecs[:, 0, :], ALU.mult)
            b.tt(tmpA[:, 0:512], kkn[:], kkn[:], ALU.mult, eng="pool")
            b.reduce(sm8[:, 0, :], tmpA[:, 0:512].rearrange("p (h d) -> p h d", d=64))
            b.ts(sm8[:, 0, :], sm8[:, 0, :], 1e-12, op0=ALU.add)
            b.act(sm8[:, 0, :], sm8[:, 0, :], AF.Sqrt)
            b.recip(sm8[:, 0, :], sm8[:, 0, :])
            k3 = kkn[:].rearrange("p (h d) -> p h d", d=64)
            b.tt(k3, k3, sm8[:, 0, :].unsqueeze(2).broadcast_to([128, 8, 64]), ALU.mult)
            for z in range(2):
                b.stt(tmpA[:, 0:512], aa[:, z * 512:(z + 1) * 512], -1.0, vecs[:, 1, :], ALU.add, ALU.mult)
                b.stt(kd[:, z, :], tmpA[:, 0:512], 1.0, kcol, ALU.add, ALU.mult)

        def rw_chunk(c, z, ST_in, ST_out):
            t0 = c * 128
            rcol, kcol, vcol = rkv[:, 0:512], rkv[:, 512:1024], rkv[:, 1024:1536]
            lw = logw[:, z * 512:(z + 1) * 512]
            b.tt(kka[:], kkn[:], aa[:, z * 512:(z + 1) * 512], ALU.mult, eng="pool")
            pcl = PA[0][:].rearrange("p a b -> p (a b)")[:, 0:512]
            ptot = PA[0][:].rearrange("p a b -> p (a b)")[:, 512:1024]
            b.mm(pcl, mk[:, z, :], lw)
            b.mm(ptot, ones_t[:], lw)
            b.copy(tots[:], ptot, eng="act")
            b.act(ex[:, 0, :], pcl, AF.Exp, scale=-1.0)
            b.act(ex[:, 1, :], pcl, AF.Exp)
            b.tt(ex[:, 2, :], pcl, lw, ALU.subtract)
            b.act(ex[:, 2, :], ex[:, 2, :], AF.Exp)
            b.tt(ex[:, 3, :], tots[:], pcl, ALU.subtract)
            b.act(ex[:, 3, :], ex[:, 3, :], AF.Exp)
            b.act(ex[0:64, 4, :], tots[0:64, :], AF.Exp)
            b.tt(tmk[:, 0, :], kd[:, z, :], ex[:, 0, :], ALU.mult)
            b.tt(tmk[:, 1, :], kka[:], ex[:, 0, :], ALU.mult, eng="pool")
            b.tt(tmk[:, 2, :], kkn[:], ex[:, 2, :], ALU.mult)
            b.tt(tmk[:, 3, :], rcol, ex[:, 1, :], ALU.mult, eng="pool")
            b.tt(tmk[:, 4, :], kd[:, z, :], ex[:, 3, :], ALU.mult)
            b.tt(tmk[:, 5, :], kka[:], ex[:, 3, :], ALU.mult, eng="pool")
            for qi in range(4):
                pq = PA[1 + qi % 2]
                for h in range(8):
                    b.tr(pq[0:64, h, :], tmk[:, qi, h * 64:(h + 1) * 64], ident_f[:])
                b.copy(fT[:, qi, :, :], pq[0:64, :, :], eng="act" if qi % 2 else "dve")
            ktT, btT, qtT, rtT = (fT[:, i, :, :] for i in range(4))
            nMs = mk[:, 2 + z, :].unsqueeze(1).broadcast_to([128, 8, 128])
            nMsT = mk[:, 3 - z, :].unsqueeze(1).broadcast_to([128, 8, 128])
            Mi = mk[:, 4 + z, :].unsqueeze(1).broadcast_to([128, 8, 128])
            F, Gm, Fp = A_["F"], A_["G"], A_["Fp"]
            Rs = [A_["R0"], A_["R1"]]
            for h in range(8):
                b.mm(PA[3][:, h, :], btT[:, h, :], qtT[:, h, :])
            b.tt(Gm[:], PA[3][:], nMs, ALU.mult)
            for h in range(8):
                b.mm(PA[0][:, h, :], qtT[:, h, :], btT[:, h, :])
            b.tt(F[:], PA[0][:], nMsT, ALU.mult)
            for h in range(8):
                b.mm(PA[1][:, h, :], ktT[:, h, :], qtT[:, h, :])
            b.tt(A_["nAkk"][:], PA[1][:], nMs, ALU.mult)
            for h in range(8):
                b.mm(PA[2][:, h, :], ktT[:, h, :], rtT[:, h, :])
            b.tt(A_["Ark"][:], PA[2][:], Mi, ALU.mult)
            for h in range(8):
                b.mm(PA[3][:, h, :], btT[:, h, :], rtT[:, h, :])
            b.tt(A_["Arb"][:], PA[3][:], Mi, ALU.mult)
            b.tt(Rs[0][:], Gm[:], i128r, ALU.add, eng="pool")
            cur = 0
            for lev in range(6):
                last = lev == 5
                for h in range(8):
                    b.mm(PA[0][:, h, :], Gm[:, h, :], F[:, h, :])
                if not last:
                    for h in range(8):
                        b.mm(PA[1][:, h, :], F[:, h, :], Gm[:, h, :])
                b.copy(F[:], PA[0][:], eng="act")
                if not last:
                    b.copy(Gm[:], PA[1][:], eng="dve")
                b.tt(Fp[:], F[:], i128r, ALU.add, eng="pool")
                for h in range(8):
                    b.mm(PA[2][:, h, :], Fp[:, h, :], Rs[cur][:, h, :])
                b.copy(Rs[1 - cur][:], PA[2][:], eng="dve" if lev % 2 else "act")
                cur = 1 - cur
            R = Rs[cur]
            pz = PA[3][:].rearrange("p a b -> p (a b)")
            for h in range(8):
                hs = slice(h * 64, (h + 1) * 64)
                b.mm(pz[:, hs], A_["nAkk"][:, h, :], vcol[:, hs])
            b.copy(Zs[:], pz[:, 0:512], eng="act")
            for h in range(8):
                hs = slice(h * 64, (h + 1) * 64)
                b.mm(pz[:, 512 + h * 64:512 + (h + 1) * 64], R[:, h, :], tmk[:, 2, hs])
            b.copy(Qh[:], pz[:, 512:1024], eng="dve")
            pu = PA[0][:].rearrange("p a b -> p (a b)")
            for h in range(8):
                hs = slice(h * 64, (h + 1) * 64)
                b.mm(pu[:, hs], R[:, h, :], Zs[:, hs])
            b.copy(nU0[:], pu[:, 0:512], eng="act")
            for h in range(8):
                hs = slice(h * 64, (h + 1) * 64)
                b.mm(pu[0:64, 512 + h * 64:512 + (h + 1) * 64], Qh[:, hs], tmk[:, 5, hs])
            b.tt(dW[:], i64r[:].rearrange("p a b -> p (a b)"), ex[0:64, 4, :], ALU.mult, eng="pool")
            b.tt(Phi[:], dW[:], pu[0:64, 512:1024], ALU.subtract)
            for h in range(8):
                hs = slice(h * 64, (h + 1) * 64)
                b.mm(PA[1][0:64, h, :], Qh[:, hs], A_["Arb"][:, h, :])
            b.tt(RhT[:], rtT, PA[1][0:64, :, :], ALU.subtract)
            py = PA[2][:].rearrange("p a b -> p (a b)")
            for h in range(8):
                hs = slice(h * 64, (h + 1) * 64)
                b.mm(py[:, hs], RhT[:, h, :], ST_in[:, h, :], start=True, stop=False)
                b.mm(py[:, hs], A_["Ark"][:, h, :], vcol[:, hs], start=False, stop=False)
                b.mm(py[:, hs], A_["Arb"][:, h, :], nU0[:, hs], start=False, stop=True)
            for h in range(8):
                hs = slice(h * 64, (h + 1) * 64)
                ps_ = py[0:64, 512 + h * 64:512 + (h + 1) * 64]
                b.mm(ps_, Phi[:, hs], ST_in[:, h, :], start=True, stop=False)
                b.mm(ps_, tmk[:, 4, hs], vcol[:, hs], start=False, stop=False)
                b.mm(ps_, tmk[:, 5, hs], nU0[:, hs], start=False, stop=True)
            b.copy(ST_out[:].rearrange("p a b -> p (a b)"), py[0:64, 512:1024], eng="act")
            rows = slice(t0, t0 + 128)
            if z == 0:
                b.copy(ysb[:], py[:, 0:512], eng="dve")
                b.dma(Orw[rows, :], ysb[:])
            else:
                b.dma(yo[:], Orw[rows, :])
                b.tt(ysb[:], py[:, 0:512], yo[:], ALU.add)
                o3 = ysb[:].rearrange("p (h d) -> p h d", d=64)
                b.reduce(sm8[:, 1, :], o3)
                b.ts(sm8[:, 1, :], sm8[:, 1, :], -1.0 / 64, op0=ALU.mult)
                b.tt(o3, o3, sm8[:, 1, :].unsqueeze(2).broadcast_to([128, 8, 64]), ALU.add)
                b.tt(tmpA[:, 0:512], ysb[:], ysb[:], ALU.mult, eng="pool")
                b.reduce(sm8[:, 2, :], tmpA[:, 0:512].rearrange("p (h d) -> p h d", d=64))
                b.rstd(sm8[:, 2, :], sm8[:, 2, :], 1.0 / 64, RW_LN_EPS)
                b.tt(o3, o3, sm8[:, 2, :].unsqueeze(2).broadcast_to([128, 8, 64]), ALU.mult)
                b.tt(ysb[:], ysb[:], vecs[:, 3, :], ALU.mult)
                b.tt(ysb[:], ysb[:], vecs[:, 4, :], ALU.add)
                b.tt(tmpA[:, 0:512], kd[:, 0, :], kd[:, 1, :], ALU.add, eng="pool")
                b.tt(tmpA[:, 0:512], tmpA[:, 0:512], vecs[:, 2, :], ALU.mult, eng="pool")
                b.tt(tmpA[:, 0:512], tmpA[:, 0:512], rcol, ALU.mult, eng="pool")
                b.reduce(sm8[:, 3, :], tmpA[:, 0:512].rearrange("p (h d) -> p h d", d=64))
                b.tt(tmpA[:, 512:1024].rearrange("p (h d) -> p h d", d=64), vcol.rearrange("p (h d) -> p h d", d=64),
                     sm8[:, 3, :].unsqueeze(2).broadcast_to([128, 8, 64]), ALU.mult)
                b.tt(ysb[:], ysb[:], tmpA[:, 512:1024], ALU.add)
                b.tt(ybf[:], ysb[:], gg[:], ALU.mult)
                b.dma(Yrw[rows, :], ybf[:])

        for z in range(2):
            order = list(range(NT)) if z == 0 else [1, 0] + list(range(NT - 1, CT - 1, -1))
            b.memset(STs[0][:], 0.0)
            cur_s = 0
            for c in order:
                rw_prep(c)
                rw_chunk(c, z, STs[cur_s], STs[1 - cur_s])
                cur_s = 1 - cur_s
        end_scope(st2)
        debug_out("Orw", Orw, [TOK, 512])
        debug_out("Yrw", Yrw, [TOK, 512], BF16)
        if cfg.stop_after == "RW":
            break

        st3 = scope()
        NRT = 16
        TWO_PI = 2.0 * math.pi
        prm = b.sb("s5_prm", [128, 3, 2, NRT])
        b.dma(prm[:], s5_par[l])
        dtt = b.sb("s5_dt", [128, 2, NRT])
        rho = b.sb("s5_rho", [128, 2, NRT])
        th = b.sb("s5_th", [128, 2, NRT])
        sc1 = b.sb("s5_sc1", [128, 8, 2, NRT])
        sci = b.sb("s5_sci", [128, 2, NRT], I32)
        b.act(dtt[:], prm[:, 2], AF.Exp)
        b.tt(rho[:], prm[:, 0], dtt[:], ALU.mult)
        b.tt(th[:], prm[:, 1], dtt[:], ALU.mult)

        def sincos(out_s, out_c, ang, tmp, tmpi, n_extra=None):
            for out_, shift in ((out_s, 0.0), (out_c, 0.5 * math.pi)):
                b.ts(tmp, ang, shift, 1.0 / TWO_PI, ALU.add, ALU.mult)
                b.copy(tmpi, tmp)
                b.copy(tmp, tmpi)
                b.stt(tmp, tmp, -TWO_PI, ang, ALU.mult, ALU.add)
                b.ts(tmp, tmp, shift, op0=ALU.add)
                b.ts(tmp, tmp, math.pi, -math.pi, ALU.min, ALU.max)
                b.act(out_, tmp, AF.Sin)

        mag = sc1[:, 0]
        b.act(mag, rho[:], AF.Exp)
        sn1, cs1 = sc1[:, 1], sc1[:, 2]
        sincos(sn1, cs1, th[:], sc1[:, 3], sci[:])
        abr, abi = sc1[:, 4], sc1[:, 5]
        b.tt(abr, mag, cs1, ALU.mult)
        b.tt(abi, mag, sn1, ALU.mult)
        den, nre = sc1[:, 6], sc1[:, 7]
        co = b.sb("s5_co", [128, 2, 2, NRT])
        tq = b.sb("s5_tq", [128, 4, 2, NRT])
        b.tt(tq[:, 0], prm[:, 0], prm[:, 0], ALU.mult)
        b.tt(tq[:, 1], prm[:, 1], prm[:, 1], ALU.mult)
        b.tt(den, tq[:, 0], tq[:, 1], ALU.add)
        b.recip(den, den)
        b.ts(nre, abr, -1.0, op0=ALU.add)
        b.tt(tq[:, 0], nre, prm[:, 0], ALU.mult)
        b.tt(tq[:, 1], abi, prm[:, 1], ALU.mult)
        b.tt(tq[:, 0], tq[:, 0], tq[:, 1], ALU.add)
        b.tt(co[:, 0], tq[:, 0], den, ALU.mult)
        b.tt(tq[:, 2], abi, prm[:, 0], ALU.mult)
        b.tt(tq[:, 3], nre, prm[:, 1], ALU.mult)
        b.tt(tq[:, 2], tq[:, 2], tq[:, 3], ALU.subtract)
        b.tt(co[:, 1], tq[:, 2], den, ALU.mult)
        cmat = b.sb("s5_cmat_sb", [128, 2, 2, NRT, 32])
        b.dma(cmat[:], s5_cmat[l])
        b.ts(cmat[:, 1], cmat[:, 1], -1.0, op0=ALU.mult, eng="pool")
        bT = b.sb("s5_bT", [32, 2, 2, NRT, 128])
        pS = [b.ps("s5_p%d" % i, [128, 512]) for i in range(6)]
        st3b = contextlib.ExitStack()
        b.st = st3b
        braw = b.sb("s5_braw", [128, 2, 2, NRT, 32])
        b.dma(braw[:], s5_bmat[l])
        bbd = b.sb("s5_bbd", [128, 2, 2, NRT, 32])
        btmp = b.sb("s5_btmp", [128, 2, NRT, 32])
        cob = lambda i: co[:, i].unsqueeze(3).broadcast_to([128, 2, NRT, 32])
        b.tt(bbd[:, 0], braw[:, 0], cob(0), ALU.mult)
        b.tt(btmp[:], braw[:, 1], cob(1), ALU.mult)
        b.tt(bbd[:, 0], bbd[:, 0], btmp[:], ALU.subtract)
        b.tt(bbd[:, 1], braw[:, 1], cob(0), ALU.mult)
        b.tt(btmp[:], braw[:, 0], cob(1), ALU.mult)
        b.tt(bbd[:, 1], bbd[:, 1], btmp[:], ALU.add)
        n_ = 0
        for ri in range(2):
            for z in range(2):
                for g4 in range(0, NRT, 4):
                    p_ = pS[n_ % 2]
                    n_ += 1
                    for k in range(4):
                        b.tr(p_[0:32, k * 128:(k + 1) * 128], bbd[:, ri, z, g4 + k, :], ident_f[:])
                    b.copy(bT[:, ri, z, g4:g4 + 4, :], p_[0:32, :].rearrange("p (a b) -> p a b", b=128), eng="act")
        st3b.close()
        S.barrier()
        b.st = st3
        iot = b.sb("s5_iota", [128, 512])
        b.dma(iot[:], iota_in.broadcast_to([128, 512]))
        ctab = b.sb("s5_ctab", [128, 2, NRT, 512], BF16) if False else None
        rot = b.sb("s5_rot", [128, 4, 2, NRT])
        ang2 = sc1[:, 3]
        b.ts(ang2, th[:], 256.0, op0=ALU.mult)
        sincos(rot[:, 1], rot[:, 0], ang2, tq[:, 0], sci[:])
        b.ts(ang2, th[:], 512.0, op0=ALU.mult)
        sincos(rot[:, 3], rot[:, 2], ang2, tq[:, 0], sci[:])
        b.act(rho[:], rho[:], AF.Exp)
        wglu = b.sb("s5_wglu", [32, NRT, 512])
        b.dma(wglu[:], s5_glu[l].rearrange("(r p) n -> p r n", p=32))
        dsk = b.sb("s5_dsk", [32, NRT])
        b.dma(dsk[:], s5_dskip[l])

        uT = b.sb("s5_uT", [32, NRT, 512])
        utok = b.sb("s5_utok", [128, 4, 512])
        tabs = b.sb("s5_tabs", [128, 2, 512])
        targ = b.sb("s5_targ", [128, 512])
        ttmp = b.sb("s5_ttmp", [128, 512])
        tti = b.sb("s5_tti", [128, 512], I32)
        mm_ = b.sb("s5_mm", [128, 4, 512])
        ww = b.sb("s5_ww", [128, 2, 512])
        zz = b.sb("s5_zz", [128, 2, 512])
        xx = b.sb("s5_xx", [128, 2, 512])
        carry = b.sb("s5_carry", [128, 2, 2, NRT])
        ctmp = b.sb("s5_ctmp", [128, 4])
        yT = b.sb("s5_yT", [32, NRT, 512])
        yprev = [b.sb("s5_yprev%d" % i, [32, 512]) for i in range(2)]
        ytk = b.sb("s5_ytk", [128, 512])
        ysg = b.sb("s5_ysg", [128, 512])
        ybf5 = b.sb("s5_ybf", [128, 512], BF16)
        b.memset(carry[:], 0.0)
        blocks = [(0, 256)] + [(256 + i * 512, 512) for i in range((TOK - 256) // 512)]
        assert blocks[-1][0] + blocks[-1][1] == TOK

        def s5_tables(z, rt):
            thc = th[:, z, rt:rt + 1]
            b.ts(targ[:], iot[:], thc, op0=ALU.mult)
            sincos(tabs[:, 1, :], tabs[:, 0, :], targ[:], ttmp[:], tti[:])

        for z in range(2):
            order = blocks if z == 0 else [blocks[0]] + blocks[:0:-1]
            for bi_, (t0, bl) in enumerate(order):
                nt_ = bl // 128
                b.dma(utok[:, :nt_, :], Ps5[t0:t0 + bl, :].rearrange("(t p) c -> p t c", p=128))
                for rt in range(NRT):
                    p_ = pS[rt % 2]
                    for k in range(nt_):
                        b.tr(p_[0:32, k * 128:(k + 1) * 128], utok[:, k, rt * 32:(rt + 1) * 32], ident_f[:])
                    dst = uT[:, rt, :bl] if z == 0 else uT[:, rt, bl - 1::-1] if False else None
                    if z == 0:
                        b.copy(uT[:, rt, :bl], p_[0:32, :bl], eng="act" if rt % 2 else "dve")
                    else:
                        b.copy(uT[:, rt, :bl][:, ::-1], p_[0:32, :bl], eng="dve")
                for rt in range(NRT):
                    s5_tables(z, rt)
                    cosT, sinT = tabs[:, 0, :bl], tabs[:, 1, :bl]
                    pbr, pbi = pS[2], pS[3]
                    b.mm(pbr[:, :bl], bT[:, 0, z, rt, :], uT[:, rt, :bl])
                    b.mm(pbi[:, :bl], bT[:, 1, z, rt, :], uT[:, rt, :bl])
                    b.tt(mm_[:, 0, :bl], pbr[:, :bl], cosT, ALU.mult)
                    b.tt(mm_[:, 1, :bl], pbi[:, :bl], sinT, ALU.mult)
                    b.tt(mm_[:, 2, :bl], pbi[:, :bl], cosT, ALU.mult)
                    b.tt(mm_[:, 3, :bl], pbr[:, :bl], sinT, ALU.mult)
                    b.tt(ww[:, 0, :bl], mm_[:, 0, :bl], mm_[:, 1, :bl], ALU.add, eng="pool")
                    b.tt(ww[:, 1, :bl], mm_[:, 2, :bl], mm_[:, 3, :bl], ALU.subtract, eng="pool")
                    rb_ = rho[:, z, rt:rt + 1].broadcast_to([128, bl])
                    b.scan(zz[:, 0, :bl], rb_, ww[:, 0, :bl], carry[:, 0, z, rt:rt + 1])
                    b.scan(zz[:, 1, :bl], rb_, ww[:, 1, :bl], carry[:, 1, z, rt:rt + 1])
                    ri = 0 if bl == 256 else 2
                    cR, sR = rot[:, ri, z, rt:rt + 1], rot[:, ri + 1, z, rt:rt + 1]
                    zre, zie = zz[:, 0, bl - 1:bl], zz[:, 1, bl - 1:bl]
                    b.tt(ctmp[:, 0:1], zre, cR, ALU.mult, eng="pool")
                    b.tt(ctmp[:, 1:2], zie, sR, ALU.mult, eng="pool")
                    b.tt(ctmp[:, 2:3], zre, sR, ALU.mult, eng="pool")
                    b.tt(ctmp[:, 3:4], zie, cR, ALU.mult, eng="pool")
                    b.tt(carry[:, 0, z, rt:rt + 1], ctmp[:, 0:1], ctmp[:, 1:2], ALU.subtract, eng="pool")
                    b.tt(carry[:, 1, z, rt:rt + 1], ctmp[:, 2:3], ctmp[:, 3:4], ALU.add, eng="pool")
                    b.tt(mm_[:, 0, :bl], zz[:, 0, :bl], cosT, ALU.mult)
                    b.tt(mm_[:, 1, :bl], zz[:, 1, :bl], sinT, ALU.mult, eng="pool")
                    b.tt(mm_[:, 2, :bl], zz[:, 0, :bl], sinT, ALU.mult)
                    b.tt(mm_[:, 3, :bl], zz[:, 1, :bl], cosT, ALU.mult, eng="pool")
                    b.tt(xx[:, 0, :bl], mm_[:, 0, :bl], mm_[:, 1, :bl], ALU.subtract)
                    b.tt(xx[:, 1, :bl], mm_[:, 2, :bl], mm_[:, 3, :bl], ALU.add, eng="pool")
                    py_ = pS[4 + rt % 2]
                    b.mm(py_[0:32, :bl], cmat[:, 0, z, rt, :], xx[:, 0, :bl], start=True, stop=False)
                    b.mm(py_[0:32, :bl], cmat[:, 1, z, rt, :], xx[:, 1, :bl], start=False, stop=True)
                    if z == 0:
                        b.copy(yT[:, rt, :bl], py_[0:32, :bl], eng="act")
                    else:
                        yp_ = yprev[rt % 2]
                        b.dma(yp_[:, :bl], Ys5T[:, rt, t0:t0 + bl])
                        b.tt(yT[:, rt, :bl][:, ::-1], py_[0:32, :bl], yp_[:, :bl][:, ::-1], ALU.add)
                if z == 0:
                    b.dma(Ys5T[:, :, t0:t0 + bl], yT[:, :, :bl])
                else:
                    for rt in range(NRT):
                        b.stt(yT[:, rt, :bl], uT[:, rt, :bl][:, ::-1], dsk[:, rt:rt + 1], yT[:, rt, :bl], ALU.mult, ALU.add)
                    yv = yT[:, :, :bl]
                    g1 = uT[:, :, :bl]
                    b.tt(g1, yv, yv, ALU.mult, eng="pool")
                    b.ts(g1, g1, 0.044715, 1.0, ALU.mult, ALU.add)
                    b.tt(g1, g1, yv, ALU.mult, eng="pool")
                    b.act(g1, g1, AF.Tanh, scale=math.sqrt(2.0 / math.pi))
                    b.ts(g1, g1, 1.0, 0.5, ALU.add, ALU.mult)
                    b.tt(yv, yv, g1, ALU.mult)
                    for k in range(nt_):
                        pz_ = pS[2 + k % 2]
                        for rt in range(NRT):
                            b.mm(pz_[:, :], yT[:, rt, k * 128:(k + 1) * 128], wglu[:, rt, :], start=(rt == 0), stop=(rt == NRT - 1))
                        pt_ = pS[k % 2]
                        for rt in range(NRT):
                            b.tr(pt_[:, rt * 32:(rt + 1) * 32], yT[:, rt, k * 128:(k + 1) * 128], ident_f[0:32, 0:32])
                        b.act(ysg[:], pz_[:], AF.Sigmoid)
                        b.tt(ybf5[:], ysg[:], pt_[:], ALU.mult)
                        b.dma(Ys5[t0 + k * 128:t0 + (k + 1) * 128, :], ybf5[:])
        end_scope(st3)
        debug_out("Ys5", Ys5, [TOK, 512], BF16)
        if cfg.stop_after == "S5":
            break

        st4 = scope()
        kTa = b.sb("at_kT", [128, AT_KV, TOK], BF16)
        b.dma(kTa[:], KTd.rearrange("h p t -> p h t"))
        vA = b.sb("at_v", [128, NT, AT_KVW], BF16)
        b.dma(vA[:], Vd.rearrange("(t p) c -> p t c", p=128))
        ones_b = b.sb("at_ones", [128, 128], BF16)
        b.memset(ones_b[:], 1.0)
        qTb = [b.sb("at_qT%d" % i, [128, AT_H, 512], BF16) for i in range(2)]
        pbuf = [b.sb("at_p%d" % i, [128, 512], BF16) for i in range(3)]
        rsum = b.sb("at_rsum", [128, 512])
        oTs = [b.sb("at_oT%d" % i, [128, 512], BF16) for i in range(2)]
        pSc = [b.ps("at_ps%d" % i, [128, 512]) for i in range(3)]
        pO = [b.ps("at_po%d" % i, [128, 512]) for i in range(2)]
        pSm = [b.ps("at_pm%d" % i, [128, 512]) for i in range(2)]
        SCALE = AT_D ** -0.5
        qblocks = [(0, 256, [0, 1])] + [(256 + i * 512, 512, list(range(NT))) for i in range((TOK - 256) // 512)]
        it = 0
        for qb, (q0, ql, keys) in enumerate(qblocks):
            qT_ = qTb[qb % 2]
            b.dma(qT_[:, :, :ql], QTd[:, :, q0:q0 + ql].rearrange("h p t -> p h t"))
            for h in range(AT_H):
                kv = h // 4
                po, pm_ = pO[h % 2], pSm[h % 2]
                for ki, kt in enumerate(keys):
                    ps_ = pSc[it % 3]
                    pb_ = pbuf[it % 3]
                    it += 1
                    b.mm(ps_[:, :ql], kTa[:, kv, kt * 128:(kt + 1) * 128], qT_[:, h, :ql])
                    b.act(pb_[:, :ql], ps_[:, :ql], AF.Exp, scale=SCALE)
                    b.mm(po[:, :ql], vA[:, kt, kv * 128:(kv + 1) * 128], pb_[:, :ql], start=(ki == 0), stop=(ki == len(keys) - 1))
                    b.mm(pm_[:, :ql], ones_b[:], pb_[:, :ql], start=(ki == 0), stop=(ki == len(keys) - 1))
                b.recip(rsum[:, :ql], pm_[:, :ql])
                o_ = oTs[h % 2]
                b.tt(o_[:, :ql], po[:, :ql], rsum[:, :ql], ALU.mult)
                b.dma(OTd[h, :, q0:q0 + ql], o_[:, :ql])
        end_scope(st4)
        debug_out("OT", OTd.rearrange("h p t -> (h p) t"), [AT_H * 128, TOK], BF16)
        if cfg.stop_after == "AT":
            break

        st5 = scope()
        wbr = b.sb("mg_wbr", [128, KC, D], BF16)
        b.dma(wbr[:], w_branch[l].rearrange("(k p) n -> p k n", p=128), eng="pool")
        gtile = [b.sb("mg_g%d" % i, [128, 3 * D]) for i in range(2)]
        ytk = [b.sb("mg_ytk%d" % i, [128, 1024], BF16) for i in range(2)]
        yTt = b.sb("mg_yT", [128, 8, 128], BF16)
        oTt = [b.sb("mg_oT%d" % i, [128, 8, 128], BF16) for i in range(2)]
        m32 = b.sb("mg_m32", [128, 512])
        t32 = b.sb("mg_t32", [128, 512])
        mbf = b.sb("mg_mbf", [128, D], BF16)
        mTs = b.sb("mg_mT", [128, KC, 128], BF16)
        pbr = [b.ps("mg_pb%d" % i, [128, 512]) for i in range(3)]
        ptr = [b.ps("mg_pt%d" % i, [128, 8, 128], BF16) for i in range(2)]
        for t in range(NT):
            rows = slice(t * 128, (t + 1) * 128)
            g_ = gtile[t % 2]
            y_ = ytk[t % 2]
            o_ = oTt[t % 2]
            b.dma(g_[:], Gd[rows, :], eng="sp")
            b.dma(y_[:, 0:512], Yrw[rows, :], eng="act")
            b.dma(y_[:, 512:1024], Ys5[rows, :], eng="act")
            b.dma(o_[:], OTd[:, :, rows].rearrange("h p t -> p h t"), eng="act")
            for k in range(8):
                b.tr(ptr[0][:, k, :], y_[:, k * 128:(k + 1) * 128], ident_b[:])
            b.copy(yTt[:], ptr[0][:], eng="act")
            for n4 in range(4):
                ns = slice(n4 * 512, (n4 + 1) * 512)
                for k in range(4):
                    b.mm(pbr[0][:], yTt[:, k, :], wbr[:, k, ns], start=(k == 0), stop=(k == 3))
                for k in range(4):
                    b.mm(pbr[1][:], yTt[:, 4 + k, :], wbr[:, 4 + k, ns], start=(k == 0), stop=(k == 3))
                for k in range(8):
                    b.mm(pbr[2][:], o_[:, k, :], wbr[:, 8 + k, ns], start=(k == 0), stop=(k == 7))
                b.tt(m32[:], pbr[0][:], g_[:, n4 * 512:(n4 + 1) * 512], ALU.mult)
                b.tt(t32[:], pbr[1][:], g_[:, D + n4 * 512:D + (n4 + 1) * 512], ALU.mult)
                b.tt(m32[:], m32[:], t32[:], ALU.add, eng="pool")
                b.tt(t32[:], pbr[2][:], g_[:, 2 * D + n4 * 512:2 * D + (n4 + 1) * 512], ALU.mult)
                b.tt(mbf[:, ns], m32[:], t32[:], ALU.add, eng="pool")
            for half in range(2):
                for k in range(8):
                    b.tr(ptr[1][:, k, :], mbf[:, (half * 8 + k) * 128:(half * 8 + k + 1) * 128], ident_b[:])
                b.copy(mTs[:, half * 8:(half + 1) * 8, :], ptr[1][:], eng="act")
            b.dma(MTd[:, :, rows].rearrange("k p t -> p k t"), mTs[:])
        end_scope(st5)
        debug_out("MT", MTd.rearrange("k p t -> (k p) t"), [D, TOK], BF16)
        if cfg.stop_after == "M1":
            break

        st6 = scope()
        wo = b.sb("m2_wo", [128, KC, D], BF16)
        b.dma(wo[:], w_out[l].rearrange("(k p) n -> p k n", p=128), eng="pool")
        rtr = b.sb("m2_rtr", [128, KC, NE])
        b.dma(rtr[:], router[l].rearrange("(k p) n -> p k n", p=128))
        gms = b.sb("m2_gms", [128, 2, D])
        G2 = b.sb("m2_G2", [128, 2, D])
        SH2 = b.sb("m2_SH2", [128, 2, D])
        g2t_ = b.sb("m2_g2t", [128, D])
        b.dma(g2t_[:], norm2_g[l].broadcast_to([128, D]))
        for w in range(2):
            b.dma(gms[:, w, :], bcast_row(modrow(l, w, 2)))
            b.dma(G2[:, w, :], bcast_row(modrow(l, w, 4)))
            b.dma(SH2[:, w, :], bcast_row(modrow(l, w, 3)))
            b.stt(G2[:, w, :], G2[:, w, :], 1.0, g2t_[:], ALU.add, ALU.mult)
        mTl = [b.sb("m2_mT%d" % i, [128, KC, 128], BF16) for i in range(2)]
        xo = [b.sb("m2_x%d" % i, [128, D]) for i in range(2)]
        h2f = b.sb("m2_h2f", [128, D])
        h2b = b.sb("m2_h2b", [128, D], BF16)
        h2Tf = b.sb("m2_h2Tf", [128, KC, 128])
        h2Tb = b.sb("m2_h2Tb", [128, KC, 128], BF16)
        t32b = b.sb("m2_t32", [128, 512])
        ss2 = b.sb("m2_ss", [128, 4])
        lg = b.sb("m2_lg", [128, NE])
        affs = b.sb("m2_aff", [128, NT, NE])
        po2 = [b.ps("m2_po%d" % i, [128, 512]) for i in range(2)]
        ptb = b.ps("m2_ptb", [128, 8, 128], BF16)
        ptf = [b.ps("m2_ptf%d" % i, [128, 4, 128]) for i in range(2)]
        plg = b.ps("m2_plg", [128, NE])
        for t in range(NT):
            rows = slice(t * 128, (t + 1) * 128)
            w = 1 if t < CT else 0
            mT_ = mTl[t % 2]
            x_ = xo[t % 2]
            b.dma(mT_[:], MTd[:, :, rows].rearrange("k p t -> p k t"), eng="act")
            b.dma(x_[:], Xcur[rows, :], eng="sp")
            for n4 in range(4):
                ns = slice(n4 * 512, (n4 + 1) * 512)
                p_ = po2[n4 % 2]
                for k in range(KC):
                    b.mm(p_[:], mT_[:, k, :], wo[:, k, ns], start=(k == 0), stop=(k == KC - 1))
                b.tt(t32b[:], p_[:], gms[:, w, ns], ALU.mult)
                b.tt(x_[:, ns], x_[:, ns], t32b[:], ALU.add, eng="pool")
            b.dma(Xcur[rows, :], x_[:])
            sc = ss2[:, 0:1]
            b.memset(sc, 0.0, eng="dve")
            b.act(h2f[:], x_[:], AF.Square, accum_out=sc)
            b.rstd(sc, sc, 1.0 / D, EPS)
            b.stt(h2f[:], x_[:], sc, G2[:, w, :], ALU.mult, ALU.mult)
            b.tt(h2f[:], h2f[:], SH2[:, w, :], ALU.add, eng="pool")
            b.copy(h2b[:], h2f[:], eng="pool")
            for half in range(2):
                for k in range(8):
                    kc = half * 8 + k
                    b.tr(ptb[:, k, :], h2b[:, kc * 128:(kc + 1) * 128], ident_b[:])
                b.copy(h2Tb[:, half * 8:(half + 1) * 8, :], ptb[:], eng="act")
            b.dma(H2Td[:, :, rows].rearrange("k p t -> p k t"), h2Tb[:])
            for q4 in range(4):
                pf = ptf[q4 % 2]
                for k in range(4):
                    kc = q4 * 4 + k
                    b.tr(pf[:, k, :], h2f[:, kc * 128:(kc + 1) * 128], ident_f[:])
                b.copy(h2Tf[:, q4 * 4:(q4 + 1) * 4, :], pf[:], eng="dve" if q4 % 2 else "act")
            for k in range(KC):
                b.mm(plg[:], h2Tf[:, k, :], rtr[:, k, :], start=(k == 0), stop=(k == KC - 1))
            b.copy(lg[:], plg[:])
            b.reduce(ss2[:, 1:2], lg[:], op=ALU.max)
            b.ts(ss2[:, 1:2], ss2[:, 1:2], -1.0, op0=ALU.mult)
            b.memset(ss2[:, 2:3], 0.0, eng="dve")
            b.act(lg[:], lg[:], AF.Exp, bias=ss2[:, 1:2], accum_out=ss2[:, 2:3])
            b.recip(ss2[:, 2:3], ss2[:, 2:3])
            b.ts(affs[:, t, :], lg[:], ss2[:, 2:3], op0=ALU.mult)
        b.dma(AFFd.rearrange("(t p) e -> p t e", p=128), affs[:])
        end_scope(st6)
        debug_out("Xmid", Xcur, [TOK, D])
        debug_out("AFF", AFFd, [TOK, NE])
        debug_out("H2T", H2Td.rearrange("k p t -> (k p) t"), [D, TOK], BF16)
        if cfg.stop_after == "M2":
            break

        st7 = scope()
        aff2 = b.sb("mo_aff", [128, NT, NE])
        b.dma(aff2[:], AFFd.rearrange("(t p) e -> p t e", p=128))
        gm = b.sb("mo_gm", [128, NT, NE])
        ones_f = b.sb("mo_ones", [128, 128])
        b.memset(ones_f[:], 1.0)
        lo = b.sb("mo_lo", [128, 2, NE])
        hi = b.sb("mo_hi", [128, 2, NE])
        mid = b.sb("mo_mid", [128, 2, NE])
        cmpb = b.sb("mo_cmp", [128, NT, NE])
        cnt = b.sb("mo_cnt", [128, 2, NE])
        gef = b.sb("mo_ge", [128, 2, NE])
        dlt = b.sb("mo_dl", [128, 2, NE])
        pcn = b.ps("mo_pc", [128, 2, NE])
        b.memset(lo[:], 0.0)
        b.memset(hi[:], 1.0)
        parts = [(0, CT, 2 * CT * 128 // NE), (CT, NT, 2 * (NT - CT) * 128 // NE)]
        for itn in range(34):
            b.tt(mid[:], lo[:], hi[:], ALU.add)
            b.ts(mid[:], mid[:], 0.5, op0=ALU.mult)
            for pi, (ta, tb_, cap) in enumerate(parts):
                n_ = tb_ - ta
                b.tt(cmpb[:, ta:tb_, :], aff2[:, ta:tb_, :], mid[:, pi, :].unsqueeze(1).broadcast_to([128, n_, NE]), ALU.is_ge)
                b.reduce(cnt[:, pi, :], cmpb[:, ta:tb_, :].rearrange("p t e -> p e t"))
            b.mm(pcn[:].rearrange("p a e -> p (a e)"), ones_f[:], cnt[:].rearrange("p a e -> p (a e)"))
            for pi, (ta, tb_, cap) in enumerate(parts):
                b.ts(gef[:, pi, :], pcn[:, pi, :], float(cap) - 0.5, op0=ALU.is_ge)
            b.tt(dlt[:], mid[:], lo[:], ALU.subtract)
            b.tt(dlt[:], dlt[:], gef[:], ALU.mult)
            b.tt(lo[:], lo[:], dlt[:], ALU.add)
            b.tt(dlt[:], hi[:], mid[:], ALU.subtract)
            b.tt(dlt[:], dlt[:], gef[:], ALU.mult)
            b.tt(hi[:], mid[:], dlt[:], ALU.add)
        for pi, (ta, tb_, cap) in enumerate(parts):
            n_ = tb_ - ta
            b.tt(cmpb[:, ta:tb_, :], aff2[:, ta:tb_, :], lo[:, pi, :].unsqueeze(1).broadcast_to([128, n_, NE]), ALU.is_ge)
        b.tt(gm[:], cmpb[:], aff2[:], ALU.mult)
        if "GM" in cfg.debug:
            o = b.out("dbg_GM", [128, NT, NE])
            b.dma(o, gm[:])

        wg = b.sb("mo_wg", [128, KC, FF], BF16)
        wu = b.sb("mo_wu", [128, KC, FF], BF16)
        wd = b.sb("mo_wd", [128, FF // 128, D], BF16)
        h2g = [b.sb("mo_h2g%d" % i, [128, KC, 512], BF16) for i in range(2)]
        hid = b.sb("mo_hid", [128, FF // 128, 512], BF16)
        sg = [b.sb("mo_sg%d" % i, [128, 512]) for i in range(2)]
        ysb2 = [b.sb("mo_y%d" % i, [128, D]) for i in range(2)]
        pgu = [b.ps("mo_pgu%d" % i, [128, 512]) for i in range(4)]
        pdn = b.ps("mo_pdn", [128, D]) if False else None
        pd4 = [b.ps("mo_pd%d" % i, [128, 512]) for i in range(3)]
        mgroups = [(0, 256)] + [(256 + i * 512, 512) for i in range((TOK - 256) // 512)]
        for e in range(NE):
            b.dma(wg[:], exp_gate[l, e].rearrange("(k p) n -> p k n", p=128), eng="pool")
            b.dma(wu[:], exp_up[l, e].rearrange("(k p) n -> p k n", p=128), eng="pool")
            b.dma(wd[:], exp_down[l, e].rearrange("(k p) n -> p k n", p=128), eng="pool")
            for gi, (t0, gl_) in enumerate(mgroups):
                hg = h2g[gi % 2]
                b.dma(hg[:, :, :gl_], H2Td[:, :, t0:t0 + gl_].rearrange("k p t -> p k t"), eng="sp")
                for fc in range(FF // 128):
                    pg_, pu_ = pgu[(fc % 2) * 2], pgu[(fc % 2) * 2 + 1]
                    for k in range(KC):
                        b.mm(pg_[:, :gl_], wg[:, k, fc * 128:(fc + 1) * 128], hg[:, k, :gl_], start=(k == 0), stop=(k == KC - 1))
                    for k in range(KC):
                        b.mm(pu_[:, :gl_], wu[:, k, fc * 128:(fc + 1) * 128], hg[:, k, :gl_], start=(k == 0), stop=(k == KC - 1))
                    s_ = sg[fc % 2]
                    b.act(s_[:, :gl_], pg_[:, :gl_], AF.Silu)
                    b.tt(hid[:, fc, :gl_], s_[:, :gl_], pu_[:, :gl_], ALU.mult)
                for j in range(gl_ // 128):
                    t = t0 // 128 + j
                    y_ = ysb2[j % 2]
                    for n4 in range(4):
                        p_ = pd4[n4 % 3]
                        for fc in range(FF // 128):
                            b.mm(p_[:], hid[:, fc, j * 128:(j + 1) * 128], wd[:, fc, n4 * 512:(n4 + 1) * 512], start=(fc == 0), stop=(fc == FF // 128 - 1))
                        if n4 % 2:
                            b.ts(y_[:, n4 * 512:(n4 + 1) * 512], p_[:], gm[:, t, e:e + 1], op0=ALU.mult)
                        else:
                            b.act(y_[:, n4 * 512:(n4 + 1) * 512], p_[:], AF.Copy, scale=gm[:, t, e:e + 1])
                    rows = slice(t * 128, (t + 1) * 128)
                    if e == 0:
                        b.dma(ACCd[rows, :], y_[:], eng="pool")
                    else:
                        dst = ACCd[rows, :]
                        S.add("pool", lambda en, dst=dst, y_=y_: en.dma_start(out=dst, in_=y_[:], accum_op=ALU.add),
                              reads=[y_[:], dst], writes=[dst], is_dma=True)
        end_scope(st7)
        st8 = scope()
        gml = b.sb("fx_g", [128, 2, D])
        for w in range(2):
            b.dma(gml[:, w, :], bcast_row(modrow(l, w, 5)))
        xa = [b.sb("fx_x%d" % i, [128, D]) for i in range(2)]
        aa_ = [b.sb("fx_a%d" % i, [128, D]) for i in range(2)]
        for t in range(NT):
            rows = slice(t * 128, (t + 1) * 128)
            w = 1 if t < CT else 0
            b.dma(xa[t % 2][:], Xcur[rows, :], eng="sp")
            b.dma(aa_[t % 2][:], ACCd[rows, :], eng="act")
            b.tt(aa_[t % 2][:], aa_[t % 2][:], gml[:, w, :], ALU.mult)
            b.tt(xa[t % 2][:], xa[t % 2][:], aa_[t % 2][:], ALU.add, eng="pool")
            b.dma(Xcur[rows, :], xa[t % 2][:])
        end_scope(st8)
        debug_out("Xout", Xcur, [TOK, D])

    if cfg.stop_after is None:
        st9 = scope()
        y_out = b.out("y_out", [cfg.L, D])
        fg = b.sb("fn_g", [128, D])
        b.dma(fg[:], final_g.broadcast_to([128, D]))
        xf = [b.sb("fn_x%d" % i, [128, D]) for i in range(2)]
        jk = b.sb("fn_jk", [128, D])
        sf = b.sb("fn_s", [128, 2])
        for t in range(CT, NT):
            x_ = xf[t % 2]
            sc = sf[:, t % 2:t % 2 + 1]
            b.dma(x_[:], Xcur[t * 128:(t + 1) * 128, :], eng="sp" if t % 2 else "act")
            b.memset(sc, 0.0, eng="dve")
            b.act(jk[:], x_[:], AF.Square, accum_out=sc)
            b.rstd(sc, sc, 1.0 / D, EPS)
            b.stt(x_[:], x_[:], sc, fg[:], ALU.mult, ALU.mult)
            b.dma(y_out[(t - CT) * 128:(t - CT + 1) * 128, :], x_[:])
        end_scope(st9)

    S.finish("sp")
    S.emit()
    return b


def _rope_tables(L):
    t = np.arange(L, dtype=np.float32)
    row = np.floor(t / GRID_W).astype(np.float32)
    col = (t - row * GRID_W).astype(np.float32)
    freqs = (np.float32(10000.0) ** (-np.arange(32, dtype=np.float32) / np.float32(32))).astype(np.float32)
    ar = (row[:, None] * freqs).astype(np.float32)
    ac = (col[:, None] * freqs).astype(np.float32)
    cs = np.concatenate([np.cos(ar), np.cos(ar), np.cos(ac), np.cos(ac)], axis=1).astype(np.float32)
    sn = np.concatenate([-np.sin(ar), np.sin(ar), -np.sin(ac), np.sin(ac)], axis=1).astype(np.float32)
    return cs, sn


def _masks():
    s = np.arange(128)[:, None]
    t = np.arange(128)[None, :]
    m = np.zeros((6, 128, 128), np.float32)
    m[0] = (s <= t)
    m[1] = (s >= t)
    m[2] = -1.0 * (s < t)
    m[3] = -1.0 * (s > t)
    m[4] = (s <= t)
    m[5] = (s >= t)
    return m


def prep_inputs(inputs, cfg, n_cores=2):
    f = lambda a: np.ascontiguousarray(np.asarray(a), dtype=np.float32)
    TOK, dl, L = cfg.TOK, cfg.depth, cfg.L
    x, c, ctx, c_ctx = f(inputs["x"]), f(inputs["c"]), f(inputs["ctx"]), f(inputs["c_ctx"])
    cs, sn = _rope_tables(L)
    rc = np.ones((TOK, 128), np.float32)
    rs = np.zeros((TOK, 128), np.float32)
    rc[CT * 128:] = cs
    rs[CT * 128:] = sn
    shared = {
        "mod_w": f(inputs["mod_w"][:dl]),
        "modb": f(inputs["mod_b"][:dl])[:, None, :],
        "norm1_g": f(inputs["norm1_g"][:dl])[:, None, :],
        "norm2_g": f(inputs["norm2_g"][:dl])[:, None, :],
        "final_g": f(inputs["final_g"])[None, :],
        "w_in": f(inputs["w_in"][:dl]),
        "ropeCS": rc, "ropeSN": rs,
        "attn_qn": f(inputs["attn_qn"][:dl])[:, None, :],
        "attn_kn": f(inputs["attn_kn"][:dl])[:, None, :],
        "ident": np.eye(128, dtype=np.float32),
        "masks": _masks(),
        "rw_conv": f(inputs["rwkv_conv"][:dl])[:, :, None, :],
        "rw_w0": f(inputs["rwkv_w0"][:dl]).reshape(dl, 1, 1024),
        "rw_a0": f(inputs["rwkv_a0"][:dl]).reshape(dl, 1, 1024),
        "rw_w2": f(inputs["rwkv_w2"][:dl]).reshape(dl, 128, 512),
        "rw_a2": f(inputs["rwkv_a2"][:dl]).reshape(dl, 128, 512),
        "rw_g2": f(inputs["rwkv_g2"][:dl]),
        "rw_kk": f(inputs["rwkv_kk"][:dl])[:, None, :],
        "rw_ka": f(inputs["rwkv_ka"][:dl])[:, None, :],
        "rw_rk": f(inputs["rwkv_rk"][:dl]).reshape(dl, 1, 512),
        "rw_lng": f(inputs["rwkv_ln_g"][:dl])[:, None, :],
        "rw_lnb": f(inputs["rwkv_ln_b"][:dl])[:, None, :],
    }
    def s5_pn(a):
        a = f(a)[:dl].reshape(dl, 2, 16, 2, 64)
        return np.ascontiguousarray(a.transpose(0, 3, 4, 1, 2).reshape(dl, 128, 2, 16))
    ldt = np.broadcast_to(f(inputs["s5_log_dt"])[:dl][:, :, :, None], (dl, 2, 32, 64))
    shared["s5_par"] = np.ascontiguousarray(np.stack([s5_pn(inputs["s5_lam_re"]), s5_pn(inputs["s5_lam_im"]), s5_pn(ldt)], axis=2))
    def s5_bd(a, bmat):
        a = f(a)[:dl]
        if not bmat:
            a = a.transpose(0, 1, 2, 4, 3)
        a = a.reshape(dl, 2, 16, 2, 64, 16)
        o = np.zeros((dl, 2, 64, 2, 16, 2, 16), np.float32)
        for gl in range(2):
            o[:, gl, :, :, :, gl, :] = a[:, :, :, gl].transpose(0, 3, 1, 2, 4)
        return o.reshape(dl, 128, 2, 16, 32)
    shared["s5_bmat"] = np.ascontiguousarray(np.stack([s5_bd(inputs["s5_b_re"], True), s5_bd(inputs["s5_b_im"], True)], axis=2))
    shared["s5_cmat"] = np.ascontiguousarray(np.stack([s5_bd(inputs["s5_c_re"], False), s5_bd(inputs["s5_c_im"], False)], axis=2))
    shared["s5_glu"] = f(inputs["s5_glu"][:dl])
    shared["s5_dskip"] = np.ascontiguousarray(f(inputs["s5_d"])[:dl].reshape(dl, 16, 32).transpose(0, 2, 1))
    shared["iota512"] = np.arange(512, dtype=np.float32)[None, :]
    for k in ("w_branch", "w_out", "router", "exp_gate", "exp_up", "exp_down"):
        shared[k] = f(inputs[k][:dl])
    maps = []
    for bi in range(n_cores):
        m = dict(shared)
        m["x_seq"] = np.concatenate([ctx[bi], x[bi]], axis=0)
        cv = np.stack([c[bi], c_ctx], 0)
        m["csT"] = np.ascontiguousarray(cv.reshape(2, D // 128, 128).transpose(2, 1, 0))
        maps.append(m)
    return maps


_CACHE = {}


def kernel(**inputs):
    cfg = Cfg()
    if "b" not in _CACHE:
        _CACHE["b"] = build(cfg)
    b = _CACHE["b"]
    maps = prep_inputs(inputs, cfg, n_cores=2)
    maps = [{k: v for k, v in m.items() if k in b.ins} for m in maps]
    res = run_bass_kernel_spmd(b.nc, maps, core_ids=[0, 1])
    out = np.stack([np.asarray(res.results[i]["y_out"], dtype=np.float32) for i in range(2)], axis=0)
    return out
```

```python
import contextlib
import math
import numpy as np
import concourse.bass as bass
import concourse.mybir as mybir
from concourse.bass_utils import run_bass_kernel_spmd

F32 = mybir.dt.float32
BF16 = mybir.dt.bfloat16
I32 = mybir.dt.int32
AF = mybir.ActivationFunctionType
ALU = mybir.AluOpType
AX = mybir.AxisListType

_DTSZ = {F32: 4, BF16: 2, I32: 4}


def dtsize(dt):
    return _DTSZ[dt]


def ap_box(ap):
    es = dtsize(ap.dtype)
    off = int(ap.offset) * es
    dims = [(int(s) * es, int(c)) for (s, c) in ap.ap if int(c) > 1 and int(s) != 0]
    name = ap.tensor.name
    if not dims:
        return (name, 0, 0, 0, off, off + es, off, off + es)
    S = max(abs(s) for s, _ in dims)
    lo = off + sum(min(0, s * (c - 1)) for s, c in dims)
    hi = off + sum(max(0, s * (c - 1)) for s, c in dims) + es
    big = [(s, c) for s, c in dims if abs(s) == S]
    rest = [(s, c) for s, c in dims if abs(s) != S]
    if len(big) == 1 and big[0][0] > 0:
        r_lo = off // S
        r_hi = r_lo + big[0][1] - 1
        c_lo = off % S + sum(min(0, s * (c - 1)) for s, c in rest)
        c_hi = off % S + sum(max(0, s * (c - 1)) for s, c in rest) + es
        if c_lo >= 0 and c_hi <= S:
            return (name, S, r_lo, r_hi, c_lo, c_hi, lo, hi)
    return (name, 0, 0, 0, lo, hi, lo, hi)


def boxes_overlap(a, b):
    if a[1] == b[1] and a[1] != 0:
        return not (a[3] < b[2] or b[3] < a[2] or a[5] <= b[4] or b[5] <= a[4])
    return not (a[7] <= b[6] or b[7] <= a[6])


def box_contains(a, b):
    if a[1] == b[1] and a[1] != 0:
        return a[2] <= b[2] and a[3] >= b[3] and a[4] <= b[4] and a[5] >= b[5]
    if a[1] == 0 and b[1] == 0:
        return a[6] <= b[6] and a[7] >= b[7]
    return False


class Op:
    __slots__ = ("eng", "fn", "idx", "waits", "signal", "tick", "is_dma", "dsem", "dval")

    def __init__(self, eng, fn, is_dma):
        self.eng = eng
        self.fn = fn
        self.is_dma = is_dma
        self.waits = {}
        self.signal = False
        self.tick = 0
        self.dsem = None
        self.dval = 0


ENGS = ("pe", "act", "dve", "pool", "sp")
N_DMA_SEMS = 32


class Sched:
    def __init__(self, nc):
        self.nc = nc
        self.ops = []
        self.by_eng = {e: [] for e in ENGS}
        self.tens = {}
        self.n_dma = 0
        self.dma_last = {}
        self.barrier_snap = None
        self.bar_done = set()

    def barrier(self):
        lasts = []
        for e in ENGS:
            for op in reversed(self.by_eng[e]):
                if not op.is_dma and op.fn is not None:
                    lasts.append(op)
                    break
        self.barrier_snap = (lasts, {k: p.dval for k, p in self.dma_last.items()})
        self.bar_done = set()

    def _need(self, op, prod):
        if prod is op:
            return
        if prod.is_dma:
            key = ("d", prod.dsem)
            val = prod.dval
            cur = op.waits.get(key)
            op.waits[key] = val if cur is None else max(cur, val)
        else:
            if prod.eng == op.eng and op.eng == "pe" and not op.is_dma:
                return
            key = ("e", prod.eng)
            prod.signal = True
            cur = op.waits.get(key)
            if cur is None or prod.idx > cur.idx:
                op.waits[key] = prod

    def add(self, eng, fn, reads=(), writes=(), is_dma=False):
        op = Op(eng, fn, is_dma)
        op.idx = len(self.ops)
        if is_dma:
            k = self.n_dma % N_DMA_SEMS
            op.dsem = k
            op.dval = 16 * (self.n_dma // N_DMA_SEMS + 1)
            prev = self.dma_last.get(k)
            if prev is not None:
                self._need(op, prev)
            self.dma_last[k] = op
            self.n_dma += 1
        rb = [ap_box(a) for a in reads]
        wb = [ap_box(a) for a in writes]
        if self.barrier_snap is not None:
            for bx in rb + wb:
                if bx[0] not in self.tens and bx[0] not in self.bar_done:
                    self.bar_done.add(bx[0])
                    for p in self.barrier_snap[0]:
                        self._need(op, p)
                    for k, v in self.barrier_snap[1].items():
                        key = ("d", k)
                        cur = op.waits.get(key)
                        op.waits[key] = v if cur is None else max(cur, v)
        for bx in rb:
            for r in self.tens.setdefault(bx[0], []):
                if r[1] == "w" and boxes_overlap(r[0], bx):
                    self._need(op, r[2])
        for bx in wb:
            for r in self.tens.setdefault(bx[0], []):
                if boxes_overlap(r[0], bx):
                    if r[1] == "r" and r[2].eng == eng and not r[2].is_dma and not is_dma:
                        continue
                    self._need(op, r[2])
        for bx in rb:
            recs = self.tens[bx[0]]
            recs[:] = [r for r in recs if not (r[1] == "r" and r[2].eng == eng and (not r[2].is_dma) and (not is_dma)
                                               and box_contains(bx, r[0]))]
            recs.append((bx, "r", op))
        for bx in wb:
            recs = self.tens[bx[0]]
            recs[:] = [r for r in recs if not box_contains(bx, r[0])]
            recs.append((bx, "w", op))
        self.ops.append(op)
        self.by_eng[eng].append(op)
        return op

    def finish(self, eng="sp"):
        op = Op(eng, None, False)
        op.idx = len(self.ops)
        for k, p in self.dma_last.items():
            op.waits[("d", k)] = p.dval
        self.ops.append(op)
        self.by_eng[eng].append(op)

    def emit(self):
        nc = self.nc
        for e in ENGS:
            t = 0
            for op in self.by_eng[e]:
                if op.signal and not op.is_dma:
                    t += 1
                    op.tick = t
        with contextlib.ExitStack() as st:
            esem = {e: st.enter_context(nc.semaphore("se_" + e)) for e in ENGS}
            dsem = [st.enter_context(nc.semaphore("sd_%d" % i)) for i in range(N_DMA_SEMS)]
            block = st.enter_context(nc.Block())
            ops_by = self.by_eng

            def run(engname, e):
                known = {}
                for op in ops_by[engname]:
                    pend = []
                    for key, val in op.waits.items():
                        if key[0] == "e":
                            v = val.tick
                            sem = esem[key[1]]
                        else:
                            v = val
                            sem = dsem[key[1]]
                        if known.get(key, 0) >= v:
                            continue
                        known[key] = v
                        pend.append((sem, v))
                    fold = None
                    if False:
                        fold = pend.pop()
                    for sem, v in pend:
                        e.wait_ge(sem, v)
                    if op.fn is None:
                        continue
                    ins = op.fn(e)
                    if fold is not None:
                        ins._wait_ge(fold[0], fold[1])
                    if op.is_dma:
                        ins.then_inc(dsem[op.dsem], 16)
                    elif op.signal:
                        ins.then_inc(esem[engname], 1)

            @block.tensor
            def _(e):
                run("pe", e)

            @block.scalar
            def _(e):
                run("act", e)

            @block.vector
            def _(e):
                run("dve", e)

            @block.gpsimd
            def _(e):
                run("pool", e)

            @block.sync
            def _(e):
                run("sp", e)


D = 2048
DEPTH = 4
N_MOD = 6
EPS = 1e-6
RW_H, RW_N, RW_W = 8, 64, 512
RW_LN_EPS = 64e-5
S5_W, S5_GRP, S5_G, S5_ST = 512, 16, 32, 64
AT_H, AT_KV, AT_D, AT_W, AT_KVW = 8, 2, 128, 1024, 256
OFF_WD = 1536
OFF_AD = OFF_WD + 128
OFF_GD = OFF_AD + 128
OFF_S5 = OFF_GD + 128
OFF_Q = OFF_S5 + 512
OFF_K = OFF_Q + 1024
OFF_V = OFF_K + 256
OFF_GATE = OFF_V + 256
N_IN = OFF_GATE + 3 * D
NTOKC = N_IN - OFF_Q
NE, FF = 16, 1024
GRID_W = 64
G = 4
GROUPS4 = [[0, 1, 2, 3], [4, 5, 6, 7]]
GROUP8 = [list(range(8))]


class Cfg:
    def __init__(self, lt=16, depth=DEPTH, stop_after=None, debug=()):
        self.LT = lt
        self.NT = lt + 1
        self.TOK = self.NT * 128
        self.depth = depth
        self.L = 4 * lt * 128
        self.CTX = 256
        self.stop_after = stop_after
        self.debug = tuple(debug)


class B:
    def __init__(self, cfg):
        self.cfg = cfg
        self.nc = bass.Bass("TRN2", target_bir_lowering=False)
        self.S = Sched(self.nc)
        self.st = contextlib.ExitStack()
        self.ins = {}
        self.outs = {}
        self._n = 0

    def inp(self, name, shape, dt=F32):
        t = self.nc.dram_tensor(name, list(shape), dt, kind="ExternalInput").ap()
        self.ins[name] = (tuple(shape), dt)
        return t

    def out(self, name, shape, dt=F32):
        t = self.nc.dram_tensor(name, list(shape), dt, kind="ExternalOutput").ap()
        self.outs[name] = (tuple(shape), dt)
        return t

    def dram(self, name, shape, dt=F32):
        return self.nc.dram_tensor(name, list(shape), dt, kind="Internal").ap()

    def sb(self, name, shape, dt=F32):
        self._n += 1
        return self.st.enter_context(self.nc.sbuf_tensor("%s_%d" % (name, self._n), list(shape), dt))

    def ps(self, name, shape, dt=F32):
        self._n += 1
        return self.st.enter_context(self.nc.psum_tensor("%s_%d" % (name, self._n), list(shape), dt))

    def dma(self, out, in_, eng="sp"):
        return self.S.add(eng, lambda e: e.dma_start(out=out, in_=in_), reads=[in_], writes=[out], is_dma=True)

    def coll(self, kind, op, groups, in_, out):
        return self.S.add("pool", lambda e: e.collective_compute(kind, op, replica_groups=groups, ins=[in_], outs=[out]),
                          reads=[in_], writes=[out], is_dma=True)

    def ag_in(self, src, dst, groups=None):
        self._n += 1
        stg = self.dram("stg%d" % self._n, list(src.shape), src.dtype)
        self.dma(stg, src, eng="act")
        return self.coll("AllGather", ALU.bypass, groups or GROUP8, stg, dst)

    def mm(self, out, lhsT, rhs, start=True, stop=True):
        return self.S.add("pe", lambda e: e.matmul(out, lhsT, rhs, start=start, stop=stop), reads=[lhsT, rhs], writes=[out])

    def tr(self, out, in_, ident):
        return self.S.add("pe", lambda e: e.transpose(out, in_, ident), reads=[in_, ident], writes=[out])

    def act(self, out, in_, func, bias=None, scale=None, accum_out=None, eng="act"):
        kw = {}
        rd = [in_]
        wr = [out]
        if bias is not None:
            kw["bias"] = bias
            if not isinstance(bias, (int, float)):
                rd.append(bias)
        if scale is not None:
            kw["scale"] = scale
            if not isinstance(scale, (int, float)):
                rd.append(scale)
        if accum_out is not None:
            kw["accum_out"] = accum_out
            wr.append(accum_out)
        return self.S.add(eng, lambda e: e.activation(out=out, in_=in_, func=func, **kw), reads=rd, writes=wr)

    def copy(self, out, in_, eng="dve"):
        if eng == "act":
            return self.S.add("act", lambda e: e.copy(out, in_), reads=[in_], writes=[out])
        return self.S.add(eng, lambda e: e.tensor_copy(out, in_), reads=[in_], writes=[out])

    def tt(self, out, in0, in1, op, eng="dve"):
        return self.S.add(eng, lambda e: e.tensor_tensor(out, in0, in1, op), reads=[in0, in1], writes=[out])

    def ts(self, out, in0, s1, s2=None, op0=ALU.mult, op1=None, eng="dve", accum_out=None):
        rd = [in0] + [s for s in (s1, s2) if s is not None and not isinstance(s, (int, float))]
        wr = [out] + ([accum_out] if accum_out is not None else [])
        if op1 is None:
            return self.S.add(eng, lambda e: e.tensor_single_scalar(out, in0, s1, op0), reads=rd, writes=wr)
        kw = {"accum_out": accum_out} if accum_out is not None else {}
        return self.S.add(eng, lambda e: e.tensor_scalar(out, in0, s1, s2, op0, op1, **kw), reads=rd, writes=wr)

    def stt(self, out, in0, scalar, in1, op0, op1, eng="dve"):
        rd = [in0, in1] + ([scalar] if not isinstance(scalar, (int, float)) else [])
        return self.S.add(eng, lambda e: e.scalar_tensor_tensor(out, in0, scalar, in1, op0, op1), reads=rd, writes=[out])

    def memset(self, out, val, eng="pool"):
        return self.S.add(eng, lambda e: e.memset(out, val), writes=[out])

    def reduce(self, out, in_, op=ALU.add, axis=AX.X, eng="dve"):
        return self.S.add(eng, lambda e: e.tensor_reduce(out, in_, axis, op), reads=[in_], writes=[out])

    def scan(self, out, d0, d1, init, op0=ALU.mult, op1=ALU.add, eng="dve"):
        rd = [d0, d1] + ([init] if not isinstance(init, (int, float)) else [])
        return self.S.add(eng, lambda e: e.tensor_tensor_scan(out, d0, d1, init, op0, op1), reads=rd, writes=[out])

    def rstd(self, out, in_, scale, eps):
        self.ts(out, in_, scale, eps, ALU.mult, ALU.add)
        self.act(out, out, AF.Sqrt)
        return self.recip(out, out)

    def recip(self, out, in_):
        return self.S.add("dve", lambda e: e.reciprocal(out, in_), reads=[in_], writes=[out])


def bcast_row(ap_row, n=128):
    return ap_row.broadcast_to([n, ap_row.shape[-1]])


CT = 2


class Cfg:
    def __init__(self, nlt=64, depth=DEPTH, stop_after=None, debug=()):
        self.NLT = nlt
        self.NT = nlt + CT
        self.TOK = self.NT * 128
        self.depth = depth
        self.L = nlt * 128
        self.stop_after = stop_after
        self.debug = tuple(debug)
        self.groups = [[0, 1]] + [list(range(CT + i, min(CT + i + 8, self.NT))) for i in range(0, nlt, 8)]


SEGS = ([(0, 512), (512, 512), (1024, 512), (1536, 384), (1920, 512), (2432, 512), (2944, 512), (3456, 512)]
        + [(OFF_GATE + i * 512, 512) for i in range(12)])


def build(cfg):
    b = B(cfg)
    nc, S = b.nc, b.S
    NT, TOK, dl = cfg.NT, cfg.TOK, cfg.depth
    KC = D // 128

    def debug_out(name, src_ap, shape, dt=F32):
        if name in cfg.debug:
            o = b.out("dbg_" + name, shape, dt)
            b.dma(o, src_ap)

    x_in = b.inp("x_seq", [TOK, D])
    csT = b.inp("csT", [128, KC, 2])
    mod_w = b.inp("mod_w", [dl, D, N_MOD * D])
    modb = b.inp("modb", [dl, 1, N_MOD * D])
    norm1_g = b.inp("norm1_g", [dl, 1, D])
    norm2_g = b.inp("norm2_g", [dl, 1, D])
    final_g = b.inp("final_g", [1, D])
    w_in = b.inp("w_in", [dl, D, N_IN])
    ropeCS = b.inp("ropeCS", [TOK, 128])
    ropeSN = b.inp("ropeSN", [TOK, 128])
    attn_qn = b.inp("attn_qn", [dl, 1, 128])
    attn_kn = b.inp("attn_kn", [dl, 1, 128])
    ident_in = b.inp("ident", [128, 128])
    masks_in = b.inp("masks", [6, 128, 128])
    rw_conv = b.inp("rw_conv", [dl, 3, 1, 1536])
    rw_w0 = b.inp("rw_w0", [dl, 1, 1024])
    rw_a0 = b.inp("rw_a0", [dl, 1, 1024])
    rw_w2 = b.inp("rw_w2", [dl, 128, 512])
    rw_a2 = b.inp("rw_a2", [dl, 128, 512])
    rw_g2 = b.inp("rw_g2", [dl, 128, 512])
    rw_kk = b.inp("rw_kk", [dl, 1, 512])
    rw_ka = b.inp("rw_ka", [dl, 1, 512])
    rw_rk = b.inp("rw_rk", [dl, 1, 512])
    rw_lng = b.inp("rw_lng", [dl, 1, 512])
    rw_lnb = b.inp("rw_lnb", [dl, 1, 512])
    s5_par = b.inp("s5_par", [dl, 128, 3, 2, 16])
    s5_bmat = b.inp("s5_bmat", [dl, 128, 2, 2, 16, 32])
    s5_cmat = b.inp("s5_cmat", [dl, 128, 2, 2, 16, 32])
    s5_glu = b.inp("s5_glu", [dl, 512, 512])
    s5_dskip = b.inp("s5_dskip", [dl, 32, 16])
    iota_in = b.inp("iota512", [1, 512])
    w_branch = b.inp("w_branch", [dl, D, D])
    w_out = b.inp("w_out", [dl, D, D])
    router = b.inp("router", [dl, D, NE])
    exp_gate = b.inp("exp_gate", [dl, NE, D, FF])
    exp_up = b.inp("exp_up", [dl, NE, D, FF])
    exp_down = b.inp("exp_down", [dl, NE, FF, D])

    pst = b.st
    ident_f = b.sb("ident_f", [128, 128])
    ident_b = b.sb("ident_b", [128, 128], BF16)
    b.dma(ident_f[:], ident_in)
    b.copy(ident_b[:], ident_f[:])
    P_idx = b.sb("P_idx", [128, NT * NE], I32)
    P_h2t = [b.sb("P_h2t%d" % i, [128, D], BF16) for i in range(1)]
    P_gb = [b.sb("P_gb%d" % i, [128, D]) for i in range(1)]

    Xcur = b.dram("Xcur", [TOK, D])
    modsel = b.dram("modsel", [dl, 2, N_MOD * D])
    Prw = b.dram("Prw", [TOK, 1920])
    Ps5 = b.dram("Ps5", [TOK, 512])
    QTd = b.dram("QTd", [AT_H, 128, TOK], BF16)
    KTd = b.dram("KTd", [AT_KV, 128, TOK], BF16)
    Vd = b.dram("Vd", [TOK, AT_KVW], BF16)
    Gd = b.dram("Gd", [TOK, 3 * D])
    Orw = b.dram("Orw", [TOK, 512])
    Yrw = b.dram("Yrw", [TOK, 512], BF16)
    Ys5T = b.dram("Ys5T", [32, 16, TOK])
    OTd = b.dram("OTd", [AT_H, 128, TOK], BF16)
    MTd = b.dram("MTd", [KC, 128, TOK], BF16)
    H2Td = b.dram("H2Td", [KC, 128, TOK], BF16)
    AFFd = b.dram("AFFd", [TOK, NE])
    H2tok = b.dram("H2tok", [TOK, D], BF16)
    CAPL_ = ((2 * (NT - CT) * 128 // NE + 127) // 128) * 128
    XSl = [b.dram("XSl%d" % e, [CAPL_ + 128, D], BF16) for e in range(NE)]
    XSc = [b.dram("XSc%d" % e, [128, D], BF16) for e in range(NE)]
    Yl = [b.dram("Yl%d" % e, [CAPL_ + 128, D]) for e in range(NE)]
    Yc = [b.dram("Yc%d" % e, [128, D]) for e in range(NE)]
    IDXd = b.dram("IDXd", [128, NT * NE], I32)
    GMd = b.dram("GMd", [128, NT, NE])
    Ys5 = b.dram("Ys5", [TOK, 512], BF16)

    def scope():
        st = contextlib.ExitStack()
        b.st = st
        return st

    def end_scope(st):
        st.close()
        S.barrier()
        b.st = pst

    st0 = scope()
    cs = b.sb("cs", [128, KC, 2])
    b.dma(cs[:], csT)
    b.act(cs[:], cs[:], AF.Silu)
    mwb = [b.sb("mwb%d" % i, [128, KC, 512]) for i in range(2)]
    mb2 = b.sb("mb2", [2, N_MOD * D])
    msel = b.sb("msel", [2, N_MOD * D])
    pm = [b.ps("pm%d" % i, [2, 512]) for i in range(2)]
    for l in range(dl):
        b.dma(mb2[:], modb[l].broadcast_to([2, N_MOD * D]))
        for n in range(N_MOD * D // 512):
            w_ = mwb[n % 2]
            b.dma(w_[:], mod_w[l][:, n * 512:(n + 1) * 512].rearrange("(k p) n -> p k n", p=128), eng="sp" if n % 2 else "act")
            p_ = pm[n % 2]
            for k in range(KC):
                b.mm(p_[:], cs[:, k, :], w_[:, k, :], start=(k == 0), stop=(k == KC - 1))
            b.tt(msel[:, n * 512:(n + 1) * 512], p_[:], mb2[:, n * 512:(n + 1) * 512], ALU.add)
        b.dma(modsel[l], msel[:])
    end_scope(st0)
    debug_out("modsel", modsel.rearrange("l r n -> (l r) n"), [dl * 2, N_MOD * D])

    def modrow(l, which, m):
        return modsel[l, which:which + 1, m * D:(m + 1) * D]

    b.dma(Xcur, x_in)

    for l in range(dl):
        st1 = scope()
        G1 = b.sb("G1", [128, 2, D])
        SH = b.sb("SH", [128, 2, D])
        gt = b.sb("gt", [128, D])
        b.dma(gt[:], norm1_g[l].broadcast_to([128, D]))
        for w in range(2):
            b.dma(G1[:, w, :], bcast_row(modrow(l, w, 1)))
            b.dma(SH[:, w, :], bcast_row(modrow(l, w, 0)))
            b.stt(G1[:, w, :], G1[:, w, :], 1.0, gt[:], ALU.add, ALU.mult)
        hT = b.sb("hT", [128, KC, 1024], BF16)
        xt = [b.sb("xtA%d" % i, [128, D]) for i in range(2)]
        hb = [b.sb("hbA%d" % i, [128, D], BF16) for i in range(2)]
        ss = b.sb("ssA", [128, 2])
        pT = [b.ps("pTA%d" % i, [128, 8, 128], BF16) for i in range(2)]
        wblk = [b.sb("wblk%d" % i, [128, KC, 512], BF16) for i in range(2)]
        pp = [b.ps("ppB%d" % i, [128, 512]) for i in range(2)]
        pTq = [b.ps("pTq%d" % i, [128, 4, 128], BF16) for i in range(2)]
        qraw = b.sb("qraw", [128, 512])
        qtmp = b.sb("qtmp", [128, 512])
        qbf = b.sb("qbf", [128, 512], BF16)
        nrm = b.sb("nrmB", [128, 4])
        qn_t = b.sb("qn_t", [128, 128])
        kn_t = b.sb("kn_t", [128, 128])
        b.dma(qn_t[:], attn_qn[l].broadcast_to([128, 128]))
        b.dma(kn_t[:], attn_kn[l].broadcast_to([128, 128]))
        cs_t = b.sb("cs_t", [128, 8, 128])
        sn_t = b.sb("sn_t", [128, 8, 128])
        osb = [b.sb("osb%d" % i, [128, 512]) for i in range(2)]
        qTs = b.sb("qTs", [128, 4, 1024], BF16)
        kTs = b.sb("kTs", [128, 2, 1024], BF16)
        vbf = b.sb("vbf", [128, 8, AT_KVW], BF16)

        def norm_rope(nheads, gain_t, j):
            W = nheads * 128
            b.act(qtmp[:, :W], qraw[:, :W], AF.Square)
            b.reduce(nrm[:, :nheads], qtmp[:, :W].rearrange("p (h d) -> p h d", d=128))
            b.rstd(nrm[:, :nheads], nrm[:, :nheads], 1.0 / 128, EPS)
            q3 = qraw[:, :W].rearrange("p (h d) -> p h d", d=128)
            b.tt(q3, q3, nrm[:, :nheads].unsqueeze(2).broadcast_to([128, nheads, 128]), ALU.mult)
            b.tt(q3, q3, gain_t[:].unsqueeze(1).broadcast_to([128, nheads, 128]), ALU.mult)
            x5 = qraw[:, :W].rearrange("p (h a f c) -> p h a f c", a=2, f=2, c=32)
            t5 = qtmp[:, :W].rearrange("p (h a f c) -> p h a f c", a=2, f=2, c=32)
            sn4 = sn_t[:, j, :].rearrange("p (a f c) -> p a f c", a=2, f=2)
            cs3 = cs_t[:, j, :].unsqueeze(1).broadcast_to([128, nheads, 128])
            for f in range(2):
                b.tt(t5[:, :, :, f, :], x5[:, :, :, 1 - f, :],
                     sn4[:, :, f, :].unsqueeze(1).broadcast_to([128, nheads, 2, 32]), ALU.mult, eng="pool")
            b.tt(q3, q3, cs3, ALU.mult)
            b.tt(qbf[:, :W], qraw[:, :W], qtmp[:, :W], ALU.add)

        for grp in cfg.groups:
            ng = len(grp)
            t0 = grp[0]
            r0, r1 = t0 * 128, (t0 + ng) * 128
            w = 1 if t0 < CT else 0
            b.dma(cs_t[:, :ng, :], ropeCS[r0:r1, :].rearrange("(t p) c -> p t c", p=128))
            b.dma(sn_t[:, :ng, :], ropeSN[r0:r1, :].rearrange("(t p) c -> p t c", p=128))
            for j, t in enumerate(grp):
                x_ = xt[j % 2]
                h_ = hb[j % 2]
                sc = ss[:, j % 2:j % 2 + 1]
                b.dma(x_[:], Xcur[t * 128:(t + 1) * 128, :], eng="sp" if j % 2 else "act")
                b.memset(sc, 0.0, eng="dve")
                b.act(h_[:], x_[:], AF.Square, accum_out=sc)
                b.rstd(sc, sc, 1.0 / D, EPS)
                b.stt(x_[:], x_[:], sc, G1[:, w, :], ALU.mult, ALU.mult)
                b.tt(h_[:], x_[:], SH[:, w, :], ALU.add, eng="pool")
                for half in range(2):
                    p_ = pT[half]
                    for k in range(8):
                        kc = half * 8 + k
                        b.tr(p_[:, k, :], h_[:, kc * 128:(kc + 1) * 128], ident_b[:])
                    b.copy(hT[:, half * 8:(half + 1) * 8, j * 128:(j + 1) * 128], p_[:], eng="act" if half else "dve")
            for si, (c0, cw) in enumerate(SEGS):
                w_ = wblk[si % 2]
                b.dma(w_[:, :, :cw], w_in[l][:, c0:c0 + cw].rearrange("(k p) n -> p k n", p=128), eng="pool")
                for j, t in enumerate(grp):
                    p_ = pp[j % 2]
                    for k in range(KC):
                        b.mm(p_[:, :cw], hT[:, k, j * 128:(j + 1) * 128], w_[:, k, :cw], start=(k == 0), stop=(k == KC - 1))
                    rows = slice(t * 128, (t + 1) * 128)
                    if si <= 3:
                        o_ = osb[j % 2]
                        b.copy(o_[:, :cw], p_[:, :cw], eng="act" if j % 2 else "dve")
                        b.dma(Prw[rows, c0:c0 + cw], o_[:, :cw])
                    elif si == 4:
                        o_ = osb[j % 2]
                        b.copy(o_[:], p_[:], eng="act" if j % 2 else "dve")
                        b.dma(Ps5[rows, :], o_[:])
                    elif si in (5, 6):
                        b.copy(qraw[:], p_[:], eng="act")
                        norm_rope(4, qn_t, j)
                        pq = pTq[j % 2]
                        for hh in range(4):
                            b.tr(pq[:, hh, :], qbf[:, hh * 128:(hh + 1) * 128], ident_b[:])
                        b.copy(qTs[:, :, j * 128:(j + 1) * 128], pq[:], eng="act")
                    elif si == 7:
                        b.copy(qraw[:], p_[:], eng="act")
                        b.copy(vbf[:, j, :], qraw[:, 256:512])
                        norm_rope(2, kn_t, j)
                        pq = pTq[j % 2]
                        for hh in range(2):
                            b.tr(pq[:, hh, :], qbf[:, hh * 128:(hh + 1) * 128], ident_b[:])
                        b.copy(kTs[:, :, j * 128:(j + 1) * 128], pq[:, 0:2, :], eng="act")
                    else:
                        o_ = osb[j % 2]
                        b.act(o_[:], p_[:], AF.Sigmoid)
                        b.dma(Gd[rows, c0 - OFF_GATE:c0 - OFF_GATE + 512], o_[:])
                if si in (5, 6):
                    hq = (si - 5) * 4
                    b.dma(QTd[hq:hq + 4, :, r0:r1].rearrange("h p t -> p h t"), qTs[:, :, :ng * 128])
                if si == 7:
                    b.dma(KTd[:, :, r0:r1].rearrange("h p t -> p h t"), kTs[:, :, :ng * 128])
                    b.dma(Vd[r0:r1, :].rearrange("(t p) c -> p t c", p=128), vbf[:, :ng, :])
        end_scope(st1)
        debug_out("Prw", Prw, [TOK, 1920])
        debug_out("Ps5", Ps5, [TOK, 512])
        debug_out("QT", QTd.rearrange("h p t -> (h p) t"), [AT_H * 128, TOK], BF16)
        debug_out("KT", KTd.rearrange("h p t -> (h p) t"), [AT_KV * 128, TOK], BF16)
        debug_out("V", Vd, [TOK, AT_KVW], BF16)
        debug_out("G", Gd, [TOK, 3 * D])
        if cfg.stop_after == "P1":
            break

        st2 = scope()
        cw = b.sb("rw_cw", [128, 3, 1536])
        for i in range(3):
            b.dma(cw[:, i, :], rw_conv[l, i].broadcast_to([128, 1536]))
        w0t = b.sb("rw_w0t", [128, 1024])
        a0t = b.sb("rw_a0t", [128, 1024])
        b.dma(w0t[:], rw_w0[l].broadcast_to([128, 1024]))
        b.dma(a0t[:], rw_a0[l].broadcast_to([128, 1024]))
        w2t = b.sb("rw_w2t", [128, 512])
        a2t = b.sb("rw_a2t", [128, 512])
        g2t = b.sb("rw_g2t", [128, 512])
        b.dma(w2t[:], rw_w2[l])
        b.dma(a2t[:], rw_a2[l])
        b.dma(g2t[:], rw_g2[l])
        vecs = b.sb("rw_vecs", [128, 5, 512])
        for i, src in enumerate((rw_kk, rw_ka, rw_rk, rw_lng, rw_lnb)):
            b.dma(vecs[:, i, :], src[l].broadcast_to([128, 512]))
        mk = b.sb("rw_mk", [128, 6, 128])
        b.dma(mk[:], masks_in.rearrange("m s t -> s m t"))
        ones_t = b.sb("rw_ones", [128, 128])
        b.memset(ones_t[:], 1.0)
        i64r = b.sb("rw_i64r", [64, 8, 64])
        b.copy(i64r[:], ident_f[0:64, 0:64].unsqueeze(1).broadcast_to([64, 8, 64]), eng="pool")
        i128r = ident_f[:].unsqueeze(1).broadcast_to([128, 8, 128])

        p3 = b.sb("rw_p3", [128, 2, 1536])
        wag = b.sb("rw_wag", [128, 384])
        rkv = b.sb("rw_rkv", [128, 1536])
        tmpA = b.sb("rw_tmpA", [128, 1536])
        lrT = b.sb("rw_lrT", [128, 3, 128])
        logw = b.sb("rw_logw", [128, 1024])
        aa = b.sb("rw_aa", [128, 1024])
        gg = b.sb("rw_gg", [128, 512])
        kkn = b.sb("rw_kkn", [128, 512])
        kd = b.sb("rw_kd", [128, 2, 512])
        kka = b.sb("rw_kka", [128, 512])
        sm8 = b.sb("rw_sm8", [128, 4, 8])
        ex = b.sb("rw_ex", [128, 5, 512])
        tots = b.sb("rw_tots", [128, 512])
        tmk = b.sb("rw_tmk", [128, 6, 512])
        fT = b.sb("rw_fT", [64, 4, 8, 128])
        A_ = {n: b.sb("rw_" + n, [128, 8, 128]) for n in ("F", "G", "Fp", "R0", "R1", "nAkk", "Ark", "Arb")}
        Zs = b.sb("rw_Zs", [128, 512])
        nU0 = b.sb("rw_nU0", [128, 512])
        Qh = b.sb("rw_Qh", [128, 512])
        Phi = b.sb("rw_Phi", [64, 512])
        dW = b.sb("rw_dW", [64, 512])
        RhT = b.sb("rw_RhT", [64, 8, 128])
        STs = [b.sb("rw_ST%d" % i, [64, 8, 64]) for i in range(2)]
        ysb = b.sb("rw_ysb", [128, 512])
        yo = b.sb("rw_yo", [128, 512])
        ybf = b.sb("rw_ybf", [128, 512], BF16)
        PA = [b.ps("rw_PA%d" % i, [128, 8, 128]) for i in range(4)]
        NEG_E = -math.exp(-0.5)

        def rw_prep(c):
            t0 = c * 128
            part_lo = 0 if c < CT else CT
            part_hi = CT - 1 if c < CT else NT - 1
            b.dma(p3[:, 1, :], Prw[t0:t0 + 128, 0:1536])
            b.dma(wag[:], Prw[t0:t0 + 128, 1536:1920], eng="act")
            if c == part_lo:
                b.memset(p3[:, 0, :], 0.0)
                b.dma(p3[1:128, 0, :], Prw[t0:t0 + 127, 0:1536])
            else:
                b.dma(p3[:, 0, :], Prw[t0 - 1:t0 + 127, 0:1536])
            b.tt(rkv[:], p3[:, 0, :], cw[:, 0, :], ALU.mult)
            if c == part_hi:
                b.memset(p3[:, 0, :], 0.0)
                b.dma(p3[0:127, 0, :], Prw[t0 + 1:t0 + 128, 0:1536], eng="act")
            else:
                b.dma(p3[:, 0, :], Prw[t0 + 1:t0 + 129, 0:1536], eng="act")
            b.tt(tmpA[:], p3[:, 1, :], cw[:, 1, :], ALU.mult, eng="pool")
            b.tt(rkv[:], rkv[:], tmpA[:], ALU.add)
            b.tt(tmpA[:], p3[:, 0, :], cw[:, 2, :], ALU.mult, eng="pool")
            b.tt(rkv[:], rkv[:], tmpA[:], ALU.add)
            b.act(wag[:, 0:128], wag[:, 0:128], AF.Tanh)
            b.act(wag[:, 256:384], wag[:, 256:384], AF.Sigmoid)
            pt = PA[0]
            for i in range(3):
                b.tr(pt[:, i, :], wag[:, i * 128:(i + 1) * 128], ident_f[:])
            b.copy(lrT[:], pt[:, 0:3, :], eng="act")
            pw = PA[1]
            pa = PA[2]
            for z in range(2):
                b.mm(pw[:, z * 4:(z + 1) * 4, :], lrT[z * 64:(z + 1) * 64, 0, :], w2t[z * 64:(z + 1) * 64, :])
                b.mm(pa[:, z * 4:(z + 1) * 4, :], lrT[z * 64:(z + 1) * 64, 1, :], a2t[z * 64:(z + 1) * 64, :])
            pg = PA[3]
            b.mm(pg[:, 0:4, :], lrT[:, 2, :], g2t[:])
            b.tt(logw[:], pw[:].rearrange("p a b -> p (a b)"), w0t[:], ALU.add)
            b.act(logw[:], logw[:], AF.Sigmoid)
            b.ts(logw[:], logw[:], NEG_E, op0=ALU.mult, eng="pool")
            b.tt(aa[:], pa[:].rearrange("p a b -> p (a b)"), a0t[:], ALU.add)
            b.act(aa[:], aa[:], AF.Sigmoid)
            b.copy(gg[:], pg[:, 0:4, :].rearrange("p a b -> p (a b)"), eng="act")
            kcol = rkv[:, 512:1024]
            b.tt(kkn[:], kcol, vecs[:, 0, :], ALU.mult)
            b.tt(tmpA[:, 0:512], kkn[:], kkn[:], ALU.mult, eng="pool")
            b.reduce(sm8[:, 0, :], tmpA[:, 0:512].rearrange("p (h d) -> p h d", d=64))
            b.ts(sm8[:, 0, :], sm8[:, 0, :], 1e-12, op0=ALU.add)
            b.act(sm8[:, 0, :], sm8[:, 0, :], AF.Sqrt)
            b.recip(sm8[:, 0, :], sm8[:, 0, :])
            k3 = kkn[:].rearrange("p (h d) -> p h d", d=64)
            b.tt(k3, k3, sm8[:, 0, :].unsqueeze(2).broadcast_to([128, 8, 64]), ALU.mult)
            for z in range(2):
                b.stt(tmpA[:, 0:512], aa[:, z * 512:(z + 1) * 512], -1.0, vecs[:, 1, :], ALU.add, ALU.mult)
                b.stt(kd[:, z, :], tmpA[:, 0:512], 1.0, kcol, ALU.add, ALU.mult)

        def rw_chunk(c, z, ST_in, ST_out):
            t0 = c * 128
            rcol, kcol, vcol = rkv[:, 0:512], rkv[:, 512:1024], rkv[:, 1024:1536]
            lw = logw[:, z * 512:(z + 1) * 512]
            b.tt(kka[:], kkn[:], aa[:, z * 512:(z + 1) * 512], ALU.mult, eng="pool")
            pcl = PA[0][:].rearrange("p a b -> p (a b)")[:, 0:512]
            ptot = PA[0][:].rearrange("p a b -> p (a b)")[:, 512:1024]
            b.mm(pcl, mk[:, z, :], lw)
            b.mm(ptot, ones_t[:], lw)
            b.copy(tots[:], ptot, eng="act")
            b.act(ex[:, 0, :], pcl, AF.Exp, scale=-1.0)
            b.act(ex[:, 1, :], pcl, AF.Exp)
            b.tt(ex[:, 2, :], pcl, lw, ALU.subtract)
            b.act(ex[:, 2, :], ex[:, 2, :], AF.Exp)
            b.tt(ex[:, 3, :], tots[:], pcl, ALU.subtract)
            b.act(ex[:, 3, :], ex[:, 3, :], AF.Exp)
            b.act(ex[0:64, 4, :], tots[0:64, :], AF.Exp)
            b.tt(tmk[:, 0, :], kd[:, z, :], ex[:, 0, :], ALU.mult)
            b.tt(tmk[:, 1, :], kka[:], ex[:, 0, :], ALU.mult, eng="pool")
            b.tt(tmk[:, 2, :], kkn[:], ex[:, 2, :], ALU.mult)
            b.tt(tmk[:, 3, :], rcol, ex[:, 1, :], ALU.mult, eng="pool")
            b.tt(tmk[:, 4, :], kd[:, z, :], ex[:, 3, :], ALU.mult)
            b.tt(tmk[:, 5, :], kka[:], ex[:, 3, :], ALU.mult, eng="pool")
            for qi in range(4):
                pq = PA[1 + qi % 2]
                for h in range(8):
                    b.tr(pq[0:64, h, :], tmk[:, qi, h * 64:(h + 1) * 64], ident_f[:])
                b.copy(fT[:, qi, :, :], pq[0:64, :, :], eng="act" if qi % 2 else "dve")
            ktT, btT, qtT, rtT = (fT[:, i, :, :] for i in range(4))
            nMs = mk[:, 2 + z, :].unsqueeze(1).broadcast_to([128, 8, 128])
            nMsT = mk[:, 3 - z, :].unsqueeze(1).broadcast_to([128, 8, 128])
            Mi = mk[:, 4 + z, :].unsqueeze(1).broadcast_to([128, 8, 128])
            F, Gm, Fp = A_["F"], A_["G"], A_["Fp"]
            Rs = [A_["R0"], A_["R1"]]
            for h in range(8):
                b.mm(PA[3][:, h, :], btT[:, h, :], qtT[:, h, :])
            b.tt(Gm[:], PA[3][:], nMs, ALU.mult)
            for h in range(8):
                b.mm(PA[0][:, h, :], qtT[:, h, :], btT[:, h, :])
            b.tt(F[:], PA[0][:], nMsT, ALU.mult)
            for h in range(8):
                b.mm(PA[1][:, h, :], ktT[:, h, :], qtT[:, h, :])
            b.tt(A_["nAkk"][:], PA[1][:], nMs, ALU.mult)
            for h in range(8):
                b.mm(PA[2][:, h, :], ktT[:, h, :], rtT[:, h, :])
            b.tt(A_["Ark"][:], PA[2][:], Mi, ALU.mult)
            for h in range(8):
                b.mm(PA[3][:, h, :], btT[:, h, :], rtT[:, h, :])
            b.tt(A_["Arb"][:], PA[3][:], Mi, ALU.mult)
            b.tt(Rs[0][:], Gm[:], i128r, ALU.add, eng="pool")
            cur = 0
            for lev in range(6):
                last = lev == 5
                for h in range(8):
                    b.mm(PA[0][:, h, :], Gm[:, h, :], F[:, h, :])
                if not last:
                    for h in range(8):
                        b.mm(PA[1][:, h, :], F[:, h, :], Gm[:, h, :])
                b.copy(F[:], PA[0][:], eng="act")
                if not last:
                    b.copy(Gm[:], PA[1][:], eng="dve")
                b.tt(Fp[:], F[:], i128r, ALU.add, eng="pool")
                for h in range(8):
                    b.mm(PA[2][:, h, :], Fp[:, h, :], Rs[cur][:, h, :])
                b.copy(Rs[1 - cur][:], PA[2][:], eng="dve" if lev % 2 else "act")
                cur = 1 - cur
            R = Rs[cur]
            pz = PA[3][:].rearrange("p a b -> p (a b)")
            for h in range(8):
                hs = slice(h * 64, (h + 1) * 64)
                b.mm(pz[:, hs], A_["nAkk"][:, h, :], vcol[:, hs])
            b.copy(Zs[:], pz[:, 0:512], eng="act")
            for h in range(8):
                hs = slice(h * 64, (h + 1) * 64)
                b.mm(pz[:, 512 + h * 64:512 + (h + 1) * 64], R[:, h, :], tmk[:, 2, hs])
            b.copy(Qh[:], pz[:, 512:1024], eng="dve")
            pu = PA[0][:].rearrange("p a b -> p (a b)")
            for h in range(8):
                hs = slice(h * 64, (h + 1) * 64)
                b.mm(pu[:, hs], R[:, h, :], Zs[:, hs])
            b.copy(nU0[:], pu[:, 0:512], eng="act")
            for h in range(8):
                hs = slice(h * 64, (h + 1) * 64)
                b.mm(pu[0:64, 512 + h * 64:512 + (h + 1) * 64], Qh[:, hs], tmk[:, 5, hs])
            b.tt(dW[:], i64r[:].rearrange("p a b -> p (a b)"), ex[0:64, 4, :], ALU.mult, eng="pool")
            b.tt(Phi[:], dW[:], pu[0:64, 512:1024], ALU.subtract)
            for h in range(8):
                hs = slice(h * 64, (h + 1) * 64)
                b.mm(PA[1][0:64, h, :], Qh[:, hs], A_["Arb"][:, h, :])
            b.tt(RhT[:], rtT, PA[1][0:64, :, :], ALU.subtract)
            py = PA[2][:].rearrange("p a b -> p (a b)")
            for h in range(8):
                hs = slice(h * 64, (h + 1) * 64)
                b.mm(py[:, hs], RhT[:, h, :], ST_in[:, h, :], start=True, stop=False)
                b.mm(py[:, hs], A_["Ark"][:, h, :], vcol[:, hs], start=False, stop=False)
                b.mm(py[:, hs], A_["Arb"][:, h, :], nU0[:, hs], start=False, stop=True)
            for h in range(8):
                hs = slice(h * 64, (h + 1) * 64)
                ps_ = py[0:64, 512 + h * 64:512 + (h + 1) * 64]
                b.mm(ps_, Phi[:, hs], ST_in[:, h, :], start=True, stop=False)
                b.mm(ps_, tmk[:, 4, hs], vcol[:, hs], start=False, stop=False)
                b.mm(ps_, tmk[:, 5, hs], nU0[:, hs], start=False, stop=True)
            b.copy(ST_out[:].rearrange("p a b -> p (a b)"), py[0:64, 512:1024], eng="act")
            rows = slice(t0, t0 + 128)
            if z == 0:
                b.copy(ysb[:], py[:, 0:512], eng="dve")
                b.dma(Orw[rows, :], ysb[:])
            else:
                b.dma(yo[:], Orw[rows, :])
                b.tt(ysb[:], py[:, 0:512], yo[:], ALU.add)
                o3 = ysb[:].rearrange("p (h d) -> p h d", d=64)
                b.reduce(sm8[:, 1, :], o3)
                b.ts(sm8[:, 1, :], sm8[:, 1, :], -1.0 / 64, op0=ALU.mult)
                b.tt(o3, o3, sm8[:, 1, :].unsqueeze(2).broadcast_to([128, 8, 64]), ALU.add)
                b.tt(tmpA[:, 0:512], ysb[:], ysb[:], ALU.mult, eng="pool")
                b.reduce(sm8[:, 2, :], tmpA[:, 0:512].rearrange("p (h d) -> p h d", d=64))
                b.rstd(sm8[:, 2, :], sm8[:, 2, :], 1.0 / 64, RW_LN_EPS)
                b.tt(o3, o3, sm8[:, 2, :].unsqueeze(2).broadcast_to([128, 8, 64]), ALU.mult)
                b.tt(ysb[:], ysb[:], vecs[:, 3, :], ALU.mult)
                b.tt(ysb[:], ysb[:], vecs[:, 4, :], ALU.add)
                b.tt(tmpA[:, 0:512], kd[:, 0, :], kd[:, 1, :], ALU.add, eng="pool")
                b.tt(tmpA[:, 0:512], tmpA[:, 0:512], vecs[:, 2, :], ALU.mult, eng="pool")
                b.tt(tmpA[:, 0:512], tmpA[:, 0:512], rcol, ALU.mult, eng="pool")
                b.reduce(sm8[:, 3, :], tmpA[:, 0:512].rearrange("p (h d) -> p h d", d=64))
                b.tt(tmpA[:, 512:1024].rearrange("p (h d) -> p h d", d=64), vcol.rearrange("p (h d) -> p h d", d=64),
                     sm8[:, 3, :].unsqueeze(2).broadcast_to([128, 8, 64]), ALU.mult)
                b.tt(ysb[:], ysb[:], tmpA[:, 512:1024], ALU.add)
                b.tt(ybf[:], ysb[:], gg[:], ALU.mult)
                b.dma(Yrw[rows, :], ybf[:])

        for z in range(2):
            order = list(range(NT)) if z == 0 else [1, 0] + list(range(NT - 1, CT - 1, -1))
            b.memset(STs[0][:], 0.0)
            cur_s = 0
            for c in order:
                rw_prep(c)
                rw_chunk(c, z, STs[cur_s], STs[1 - cur_s])
                cur_s = 1 - cur_s
        end_scope(st2)
        debug_out("Orw", Orw, [TOK, 512])
        debug_out("Yrw", Yrw, [TOK, 512], BF16)
        if cfg.stop_after == "RW":
            break

        st3 = scope()
        NRT = 16
        TWO_PI = 2.0 * math.pi
        prm = b.sb("s5_prm", [128, 3, 2, NRT])
        b.dma(prm[:], s5_par[l])
        dtt = b.sb("s5_dt", [128, 2, NRT])
        rho = b.sb("s5_rho", [128, 2, NRT])
        th = b.sb("s5_th", [128, 2, NRT])
        sc1 = b.sb("s5_sc1", [128, 8, 2, NRT])
        sci = b.sb("s5_sci", [128, 2, NRT], I32)
        b.act(dtt[:], prm[:, 2], AF.Exp)
        b.tt(rho[:], prm[:, 0], dtt[:], ALU.mult)
        b.tt(th[:], prm[:, 1], dtt[:], ALU.mult)

        def sincos(out_s, out_c, ang, tmp, tmpi, n_extra=None):
            for out_, shift in ((out_s, 0.0), (out_c, 0.5 * math.pi)):
                b.ts(tmp, ang, shift, 1.0 / TWO_PI, ALU.add, ALU.mult)
                b.copy(tmpi, tmp)
                b.copy(tmp, tmpi)
                b.stt(tmp, tmp, -TWO_PI, ang, ALU.mult, ALU.add)
                b.ts(tmp, tmp, shift, op0=ALU.add)
                b.ts(tmp, tmp, math.pi, -math.pi, ALU.min, ALU.max)
                b.act(out_, tmp, AF.Sin)

        mag = sc1[:, 0]
        b.act(mag, rho[:], AF.Exp)
        sn1, cs1 = sc1[:, 1], sc1[:, 2]
        sincos(sn1, cs1, th[:], sc1[:, 3], sci[:])
        abr, abi = sc1[:, 4], sc1[:, 5]
        b.tt(abr, mag, cs1, ALU.mult)
        b.tt(abi, mag, sn1, ALU.mult)
        den, nre = sc1[:, 6], sc1[:, 7]
        co = b.sb("s5_co", [128, 2, 2, NRT])
        tq = b.sb("s5_tq", [128, 4, 2, NRT])
        b.tt(tq[:, 0], prm[:, 0], prm[:, 0], ALU.mult)
        b.tt(tq[:, 1], prm[:, 1], prm[:, 1], ALU.mult)
        b.tt(den, tq[:, 0], tq[:, 1], ALU.add)
        b.recip(den, den)
        b.ts(nre, abr, -1.0, op0=ALU.add)
        b.tt(tq[:, 0], nre, prm[:, 0], ALU.mult)
        b.tt(tq[:, 1], abi, prm[:, 1], ALU.mult)
        b.tt(tq[:, 0], tq[:, 0], tq[:, 1], ALU.add)
        b.tt(co[:, 0], tq[:, 0], den, ALU.mult)
        b.tt(tq[:, 2], abi, prm[:, 0], ALU.mult)
        b.tt(tq[:, 3], nre, prm[:, 1], ALU.mult)
        b.tt(tq[:, 2], tq[:, 2], tq[:, 3], ALU.subtract)
        b.tt(co[:, 1], tq[:, 2], den, ALU.mult)
        cmat = b.sb("s5_cmat_sb", [128, 2, 2, NRT, 32])
        b.dma(cmat[:], s5_cmat[l])
        b.ts(cmat[:, 1], cmat[:, 1], -1.0, op0=ALU.mult, eng="pool")
        bT = b.sb("s5_bT", [32, 2, 2, NRT, 128])
        pS = [b.ps("s5_p%d" % i, [128, 512]) for i in range(6)]
        st3b = contextlib.ExitStack()
        b.st = st3b
        braw = b.sb("s5_braw", [128, 2, 2, NRT, 32])
        b.dma(braw[:], s5_bmat[l])
        bbd = b.sb("s5_bbd", [128, 2, 2, NRT, 32])
        btmp = b.sb("s5_btmp", [128, 2, NRT, 32])
        cob = lambda i: co[:, i].unsqueeze(3).broadcast_to([128, 2, NRT, 32])
        b.tt(bbd[:, 0], braw[:, 0], cob(0), ALU.mult)
        b.tt(btmp[:], braw[:, 1], cob(1), ALU.mult)
        b.tt(bbd[:, 0], bbd[:, 0], btmp[:], ALU.subtract)
        b.tt(bbd[:, 1], braw[:, 1], cob(0), ALU.mult)
        b.tt(btmp[:], braw[:, 0], cob(1), ALU.mult)
        b.tt(bbd[:, 1], bbd[:, 1], btmp[:], ALU.add)
        n_ = 0
        for ri in range(2):
            for z in range(2):
                for g4 in range(0, NRT, 4):
                    p_ = pS[n_ % 2]
                    n_ += 1
                    for k in range(4):
                        b.tr(p_[0:32, k * 128:(k + 1) * 128], bbd[:, ri, z, g4 + k, :], ident_f[:])
                    b.copy(bT[:, ri, z, g4:g4 + 4, :], p_[0:32, :].rearrange("p (a b) -> p a b", b=128), eng="act")
        st3b.close()
        S.barrier()
        b.st = st3
        iot = b.sb("s5_iota", [128, 512])
        b.dma(iot[:], iota_in.broadcast_to([128, 512]))
        ctab = b.sb("s5_ctab", [128, 2, NRT, 512], BF16) if False else None
        rot = b.sb("s5_rot", [128, 4, 2, NRT])
        ang2 = sc1[:, 3]
        b.ts(ang2, th[:], 256.0, op0=ALU.mult)
        sincos(rot[:, 1], rot[:, 0], ang2, tq[:, 0], sci[:])
        b.ts(ang2, th[:], 512.0, op0=ALU.mult)
        sincos(rot[:, 3], rot[:, 2], ang2, tq[:, 0], sci[:])
        b.act(rho[:], rho[:], AF.Exp)
        wglu = b.sb("s5_wglu", [32, NRT, 512])
        b.dma(wglu[:], s5_glu[l].rearrange("(r p) n -> p r n", p=32))
        dsk = b.sb("s5_dsk", [32, NRT])
        b.dma(dsk[:], s5_dskip[l])

        uT = b.sb("s5_uT", [32, NRT, 512])
        utok = b.sb("s5_utok", [128, 4, 512])
        tabs = b.sb("s5_tabs", [128, 2, 512])
        targ = b.sb("s5_targ", [128, 512])
        ttmp = b.sb("s5_ttmp", [128, 512])
        tti = b.sb("s5_tti", [128, 512], I32)
        mm_ = b.sb("s5_mm", [128, 4, 512])
        ww = b.sb("s5_ww", [128, 2, 512])
        zz = b.sb("s5_zz", [128, 2, 512])
        xx = b.sb("s5_xx", [128, 2, 512])
        carry = b.sb("s5_carry", [128, 2, 2, NRT])
        ctmp = b.sb("s5_ctmp", [128, 4])
        yT = b.sb("s5_yT", [32, NRT, 512])
        yprev = [b.sb("s5_yprev%d" % i, [32, 512]) for i in range(2)]
        ytk = b.sb("s5_ytk", [128, 512])
        ysg = b.sb("s5_ysg", [128, 512])
        ybf5 = b.sb("s5_ybf", [128, 512], BF16)
        b.memset(carry[:], 0.0)
        blocks = [(0, 256)] + [(256 + i * 512, 512) for i in range((TOK - 256) // 512)]
        assert blocks[-1][0] + blocks[-1][1] == TOK

        def s5_tables(z, rt):
            thc = th[:, z, rt:rt + 1]
            b.ts(targ[:], iot[:], thc, op0=ALU.mult)
            sincos(tabs[:, 1, :], tabs[:, 0, :], targ[:], ttmp[:], tti[:])

        for z in range(2):
            order = blocks if z == 0 else [blocks[0]] + blocks[:0:-1]
            for bi_, (t0, bl) in enumerate(order):
                nt_ = bl // 128
                b.dma(utok[:, :nt_, :], Ps5[t0:t0 + bl, :].rearrange("(t p) c -> p t c", p=128))
                for rt in range(NRT):
                    p_ = pS[rt % 2]
                    for k in range(nt_):
                        b.tr(p_[0:32, k * 128:(k + 1) * 128], utok[:, k, rt * 32:(rt + 1) * 32], ident_f[:])
                    dst = uT[:, rt, :bl] if z == 0 else uT[:, rt, bl - 1::-1] if False else None
                    if z == 0:
                        b.copy(uT[:, rt, :bl], p_[0:32, :bl], eng="act" if rt % 2 else "dve")
                    else:
                        b.copy(uT[:, rt, :bl][:, ::-1], p_[0:32, :bl], eng="dve")
                for rt in range(NRT):
                    s5_tables(z, rt)
                    cosT, sinT = tabs[:, 0, :bl], tabs[:, 1, :bl]
                    pbr, pbi = pS[2], pS[3]
                    b.mm(pbr[:, :bl], bT[:, 0, z, rt, :], uT[:, rt, :bl])
                    b.mm(pbi[:, :bl], bT[:, 1, z, rt, :], uT[:, rt, :bl])
                    b.tt(mm_[:, 0, :bl], pbr[:, :bl], cosT, ALU.mult)
                    b.tt(mm_[:, 1, :bl], pbi[:, :bl], sinT, ALU.mult)
                    b.tt(mm_[:, 2, :bl], pbi[:, :bl], cosT, ALU.mult)
                    b.tt(mm_[:, 3, :bl], pbr[:, :bl], sinT, ALU.mult)
                    b.tt(ww[:, 0, :bl], mm_[:, 0, :bl], mm_[:, 1, :bl], ALU.add, eng="pool")
                    b.tt(ww[:, 1, :bl], mm_[:, 2, :bl], mm_[:, 3, :bl], ALU.subtract, eng="pool")
                    rb_ = rho[:, z, rt:rt + 1].broadcast_to([128, bl])
                    b.scan(zz[:, 0, :bl], rb_, ww[:, 0, :bl], carry[:, 0, z, rt:rt + 1])
                    b.scan(zz[:, 1, :bl], rb_, ww[:, 1, :bl], carry[:, 1, z, rt:rt + 1])
                    ri = 0 if bl == 256 else 2
                    cR, sR = rot[:, ri, z, rt:rt + 1], rot[:, ri + 1, z, rt:rt + 1]
                    zre, zie = zz[:, 0, bl - 1:bl], zz[:, 1, bl - 1:bl]
                    b.tt(ctmp[:, 0:1], zre, cR, ALU.mult, eng="pool")
                    b.tt(ctmp[:, 1:2], zie, sR, ALU.mult, eng="pool")
                    b.tt(ctmp[:, 2:3], zre, sR, ALU.mult, eng="pool")
                    b.tt(ctmp[:, 3:4], zie, cR, ALU.mult, eng="pool")
                    b.tt(carry[:, 0, z, rt:rt + 1], ctmp[:, 0:1], ctmp[:, 1:2], ALU.subtract, eng="pool")
                    b.tt(carry[:, 1, z, rt:rt + 1], ctmp[:, 2:3], ctmp[:, 3:4], ALU.add, eng="pool")
                    b.tt(mm_[:, 0, :bl], zz[:, 0, :bl], cosT, ALU.mult)
                    b.tt(mm_[:, 1, :bl], zz[:, 1, :bl], sinT, ALU.mult, eng="pool")
                    b.tt(mm_[:, 2, :bl], zz[:, 0, :bl], sinT, ALU.mult)
                    b.tt(mm_[:, 3, :bl], zz[:, 1, :bl], cosT, ALU.mult, eng="pool")
                    b.tt(xx[:, 0, :bl], mm_[:, 0, :bl], mm_[:, 1, :bl], ALU.subtract)
                    b.tt(xx[:, 1, :bl], mm_[:, 2, :bl], mm_[:, 3, :bl], ALU.add, eng="pool")
                    py_ = pS[4 + rt % 2]
                    b.mm(py_[0:32, :bl], cmat[:, 0, z, rt, :], xx[:, 0, :bl], start=True, stop=False)
                    b.mm(py_[0:32, :bl], cmat[:, 1, z, rt, :], xx[:, 1, :bl], start=False, stop=True)
                    if z == 0:
                        b.copy(yT[:, rt, :bl], py_[0:32, :bl], eng="act")
                    else:
                        yp_ = yprev[rt % 2]
                        b.dma(yp_[:, :bl], Ys5T[:, rt, t0:t0 + bl])
                        b.tt(yT[:, rt, :bl][:, ::-1], py_[0:32, :bl], yp_[:, :bl][:, ::-1], ALU.add)
                if z == 0:
                    b.dma(Ys5T[:, :, t0:t0 + bl], yT[:, :, :bl])
                else:
                    for rt in range(NRT):
                        b.stt(yT[:, rt, :bl], uT[:, rt, :bl][:, ::-1], dsk[:, rt:rt + 1], yT[:, rt, :bl], ALU.mult, ALU.add)
                    yv = yT[:, :, :bl]
                    g1 = uT[:, :, :bl]
                    b.tt(g1, yv, yv, ALU.mult, eng="pool")
                    b.ts(g1, g1, 0.044715, 1.0, ALU.mult, ALU.add)
                    b.tt(g1, g1, yv, ALU.mult, eng="pool")
                    b.act(g1, g1, AF.Tanh, scale=math.sqrt(2.0 / math.pi))
                    b.ts(g1, g1, 1.0, 0.5, ALU.add, ALU.mult)
                    b.tt(yv, yv, g1, ALU.mult)
                    for k in range(nt_):
                        pz_ = pS[2 + k % 2]
                        for rt in range(NRT):
                            b.mm(pz_[:, :], yT[:, rt, k * 128:(k + 1) * 128], wglu[:, rt, :], start=(rt == 0), stop=(rt == NRT - 1))
                        pt_ = pS[k % 2]
                        for rt in range(NRT):
                            b.tr(pt_[:, rt * 32:(rt + 1) * 32], yT[:, rt, k * 128:(k + 1) * 128], ident_f[0:32, 0:32])
                        b.act(ysg[:], pz_[:], AF.Sigmoid)
                        b.tt(ybf5[:], ysg[:], pt_[:], ALU.mult)
                        b.dma(Ys5[t0 + k * 128:t0 + (k + 1) * 128, :], ybf5[:])
        end_scope(st3)
        debug_out("Ys5", Ys5, [TOK, 512], BF16)
        if cfg.stop_after == "S5":
            break

        st4 = scope()
        kTa = b.sb("at_kT", [128, AT_KV, TOK], BF16)
        b.dma(kTa[:], KTd.rearrange("h p t -> p h t"))
        vA = b.sb("at_v", [128, NT, AT_KVW], BF16)
        b.dma(vA[:], Vd.rearrange("(t p) c -> p t c", p=128))
        ones_b = b.sb("at_ones", [128, 128], BF16)
        b.memset(ones_b[:], 1.0)
        qTb = [b.sb("at_qT%d" % i, [128, AT_H, 512], BF16) for i in range(2)]
        pbuf = [b.sb("at_p%d" % i, [128, 512], BF16) for i in range(3)]
        rsum = b.sb("at_rsum", [128, 512])
        oTs = [b.sb("at_oT%d" % i, [128, 512], BF16) for i in range(2)]
        pSc = [b.ps("at_ps%d" % i, [128, 512]) for i in range(3)]
        pO = [b.ps("at_po%d" % i, [128, 512]) for i in range(2)]
        pSm = [b.ps("at_pm%d" % i, [128, 512]) for i in range(2)]
        SCALE = AT_D ** -0.5
        qblocks = [(0, 256, [0, 1])] + [(256 + i * 512, 512, list(range(NT))) for i in range((TOK - 256) // 512)]
        it = 0
        for qb, (q0, ql, keys) in enumerate(qblocks):
            qT_ = qTb[qb % 2]
            b.dma(qT_[:, :, :ql], QTd[:, :, q0:q0 + ql].rearrange("h p t -> p h t"))
            for h in range(AT_H):
                kv = h // 4
                po, pm_ = pO[h % 2], pSm[h % 2]
                for ki, kt in enumerate(keys):
                    ps_ = pSc[it % 3]
                    pb_ = pbuf[it % 3]
                    it += 1
                    b.mm(ps_[:, :ql], kTa[:, kv, kt * 128:(kt + 1) * 128], qT_[:, h, :ql])
                    b.act(pb_[:, :ql], ps_[:, :ql], AF.Exp, scale=SCALE)
                    b.mm(po[:, :ql], vA[:, kt, kv * 128:(kv + 1) * 128], pb_[:, :ql], start=(ki == 0), stop=(ki == len(keys) - 1))
                    b.mm(pm_[:, :ql], ones_b[:], pb_[:, :ql], start=(ki == 0), stop=(ki == len(keys) - 1))
                b.recip(rsum[:, :ql], pm_[:, :ql])
                o_ = oTs[h % 2]
                b.tt(o_[:, :ql], po[:, :ql], rsum[:, :ql], ALU.mult)
                b.dma(OTd[h, :, q0:q0 + ql], o_[:, :ql])
        end_scope(st4)
        debug_out("OT", OTd.rearrange("h p t -> (h p) t"), [AT_H * 128, TOK], BF16)
        if cfg.stop_after == "AT":
            break

        st5 = scope()
        wbr = b.sb("mg_wbr", [128, KC, D], BF16)
        b.dma(wbr[:], w_branch[l].rearrange("(k p) n -> p k n", p=128), eng="pool")
        gtile = [b.sb("mg_g%d" % i, [128, 3 * D]) for i in range(2)]
        ytk = [b.sb("mg_ytk%d" % i, [128, 1024], BF16) for i in range(2)]
        yTt = b.sb("mg_yT", [128, 8, 128], BF16)
        oTt = [b.sb("mg_oT%d" % i, [128, 8, 128], BF16) for i in range(2)]
        m32 = b.sb("mg_m32", [128, 512])
        t32 = b.sb("mg_t32", [128, 512])
        mbf = b.sb("mg_mbf", [128, D], BF16)
        mTs = b.sb("mg_mT", [128, KC, 128], BF16)
        pbr = [b.ps("mg_pb%d" % i, [128, 512]) for i in range(3)]
        ptr = [b.ps("mg_pt%d" % i, [128, 8, 128], BF16) for i in range(2)]
        for t in range(NT):
            rows = slice(t * 128, (t + 1) * 128)
            g_ = gtile[t % 2]
            y_ = ytk[t % 2]
            o_ = oTt[t % 2]
            b.dma(g_[:], Gd[rows, :], eng="sp")
            b.dma(y_[:, 0:512], Yrw[rows, :], eng="act")
            b.dma(y_[:, 512:1024], Ys5[rows, :], eng="act")
            b.dma(o_[:], OTd[:, :, rows].rearrange("h p t -> p h t"), eng="act")
            for k in range(8):
                b.tr(ptr[0][:, k, :], y_[:, k * 128:(k + 1) * 128], ident_b[:])
            b.copy(yTt[:], ptr[0][:], eng="act")
            for n4 in range(4):
                ns = slice(n4 * 512, (n4 + 1) * 512)
                for k in range(4):
                    b.mm(pbr[0][:], yTt[:, k, :], wbr[:, k, ns], start=(k == 0), stop=(k == 3))
                for k in range(4):
                    b.mm(pbr[1][:], yTt[:, 4 + k, :], wbr[:, 4 + k, ns], start=(k == 0), stop=(k == 3))
                for k in range(8):
                    b.mm(pbr[2][:], o_[:, k, :], wbr[:, 8 + k, ns], start=(k == 0), stop=(k == 7))
                b.tt(m32[:], pbr[0][:], g_[:, n4 * 512:(n4 + 1) * 512], ALU.mult)
                b.tt(t32[:], pbr[1][:], g_[:, D + n4 * 512:D + (n4 + 1) * 512], ALU.mult)
                b.tt(m32[:], m32[:], t32[:], ALU.add, eng="pool")
                b.tt(t32[:], pbr[2][:], g_[:, 2 * D + n4 * 512:2 * D + (n4 + 1) * 512], ALU.mult)
                b.tt(mbf[:, ns], m32[:], t32[:], ALU.add, eng="pool")
            for half in range(2):
                for k in range(8):
                    b.tr(ptr[1][:, k, :], mbf[:, (half * 8 + k) * 128:(half * 8 + k + 1) * 128], ident_b[:])
                b.copy(mTs[:, half * 8:(half + 1) * 8, :], ptr[1][:], eng="act")
            b.dma(MTd[:, :, rows].rearrange("k p t -> p k t"), mTs[:])
        end_scope(st5)
        debug_out("MT", MTd.rearrange("k p t -> (k p) t"), [D, TOK], BF16)
        if cfg.stop_after == "M1":
            break

        st6 = scope()
        wo = b.sb("m2_wo", [128, KC, D], BF16)
        b.dma(wo[:], w_out[l].rearrange("(k p) n -> p k n", p=128), eng="pool")
        rtr = b.sb("m2_rtr", [128, KC, NE])
        b.dma(rtr[:], router[l].rearrange("(k p) n -> p k n", p=128))
        gms = b.sb("m2_gms", [128, 2, D])
        G2 = b.sb("m2_G2", [128, 2, D])
        SH2 = b.sb("m2_SH2", [128, 2, D])
        g2t_ = b.sb("m2_g2t", [128, D])
        b.dma(g2t_[:], norm2_g[l].broadcast_to([128, D]))
        for w in range(2):
            b.dma(gms[:, w, :], bcast_row(modrow(l, w, 2)))
            b.dma(G2[:, w, :], bcast_row(modrow(l, w, 4)))
            b.dma(SH2[:, w, :], bcast_row(modrow(l, w, 3)))
            b.stt(G2[:, w, :], G2[:, w, :], 1.0, g2t_[:], ALU.add, ALU.mult)
        mTl = [b.sb("m2_mT%d" % i, [128, KC, 128], BF16) for i in range(2)]
        xo = [b.sb("m2_x%d" % i, [128, D]) for i in range(2)]
        h2f = b.sb("m2_h2f", [128, D])
        h2b = b.sb("m2_h2b", [128, D], BF16)
        h2Tf = b.sb("m2_h2Tf", [128, KC, 128])
        h2Tb = b.sb("m2_h2Tb", [128, KC, 128], BF16)
        t32b = b.sb("m2_t32", [128, 512])
        ss2 = b.sb("m2_ss", [128, 4])
        lg = b.sb("m2_lg", [128, NE])
        affs = b.sb("m2_aff", [128, NT, NE])
        po2 = [b.ps("m2_po%d" % i, [128, 512]) for i in range(2)]
        ptb = b.ps("m2_ptb", [128, 8, 128], BF16)
        ptf = [b.ps("m2_ptf%d" % i, [128, 4, 128]) for i in range(2)]
        plg = b.ps("m2_plg", [128, NE])
        for t in range(NT):
            rows = slice(t * 128, (t + 1) * 128)
            w = 1 if t < CT else 0
            mT_ = mTl[t % 2]
            x_ = xo[t % 2]
            b.dma(mT_[:], MTd[:, :, rows].rearrange("k p t -> p k t"), eng="act")
            b.dma(x_[:], Xcur[rows, :], eng="sp")
            for n4 in range(4):
                ns = slice(n4 * 512, (n4 + 1) * 512)
                p_ = po2[n4 % 2]
                for k in range(KC):
                    b.mm(p_[:], mT_[:, k, :], wo[:, k, ns], start=(k == 0), stop=(k == KC - 1))
                b.tt(t32b[:], p_[:], gms[:, w, ns], ALU.mult)
                b.tt(x_[:, ns], x_[:, ns], t32b[:], ALU.add, eng="pool")
            b.dma(Xcur[rows, :], x_[:])
            sc = ss2[:, 0:1]
            b.memset(sc, 0.0, eng="dve")
            b.act(h2f[:], x_[:], AF.Square, accum_out=sc)
            b.rstd(sc, sc, 1.0 / D, EPS)
            b.stt(h2f[:], x_[:], sc, G2[:, w, :], ALU.mult, ALU.mult)
            b.tt(h2f[:], h2f[:], SH2[:, w, :], ALU.add, eng="pool")
            b.copy(h2b[:], h2f[:], eng="pool")
            b.dma(H2tok[rows, :], h2b[:])
            for q4 in range(4):
                pf = ptf[q4 % 2]
                for k in range(4):
                    kc = q4 * 4 + k
                    b.tr(pf[:, k, :], h2f[:, kc * 128:(kc + 1) * 128], ident_f[:])
                b.copy(h2Tf[:, q4 * 4:(q4 + 1) * 4, :], pf[:], eng="dve" if q4 % 2 else "act")
            for k in range(KC):
                b.mm(plg[:], h2Tf[:, k, :], rtr[:, k, :], start=(k == 0), stop=(k == KC - 1))
            b.copy(lg[:], plg[:])
            b.reduce(ss2[:, 1:2], lg[:], op=ALU.max)
            b.ts(ss2[:, 1:2], ss2[:, 1:2], -1.0, op0=ALU.mult)
            b.memset(ss2[:, 2:3], 0.0, eng="dve")
            b.act(lg[:], lg[:], AF.Exp, bias=ss2[:, 1:2], accum_out=ss2[:, 2:3])
            b.recip(ss2[:, 2:3], ss2[:, 2:3])
            b.ts(affs[:, t, :], lg[:], ss2[:, 2:3], op0=ALU.mult)
        b.dma(AFFd.rearrange("(t p) e -> p t e", p=128), affs[:])
        end_scope(st6)
        debug_out("Xmid", Xcur, [TOK, D])
        debug_out("AFF", AFFd, [TOK, NE])
        debug_out("H2T", H2Td.rearrange("k p t -> (k p) t"), [D, TOK], BF16)
        if cfg.stop_after == "M2":
            break

        st7 = scope()
        aff2 = b.sb("mo_aff", [128, NT, NE])
        b.dma(aff2[:], AFFd.rearrange("(t p) e -> p t e", p=128))
        gm = b.sb("mo_gm", [128, NT, NE])
        ones_f = b.sb("mo_ones", [128, 128])
        b.memset(ones_f[:], 1.0)
        lo = b.sb("mo_lo", [128, 2, NE])
        hi = b.sb("mo_hi", [128, 2, NE])
        mid = b.sb("mo_mid", [128, 2, NE])
        cmpb = b.sb("mo_cmp", [128, NT, NE])
        cnt = b.sb("mo_cnt", [128, 2, NE])
        gef = b.sb("mo_ge", [128, 2, NE])
        dlt = b.sb("mo_dl", [128, 2, NE])
        pcn = b.ps("mo_pc", [128, 2, NE])
        b.memset(lo[:], 0.0)
        b.memset(hi[:], 1.0)
        parts = [(0, CT, 2 * CT * 128 // NE), (CT, NT, 2 * (NT - CT) * 128 // NE)]
        for itn in range(34):
            b.tt(mid[:], lo[:], hi[:], ALU.add)
            b.ts(mid[:], mid[:], 0.5, op0=ALU.mult)
            for pi, (ta, tb_, cap) in enumerate(parts):
                n_ = tb_ - ta
                b.tt(cmpb[:, ta:tb_, :], aff2[:, ta:tb_, :], mid[:, pi, :].unsqueeze(1).broadcast_to([128, n_, NE]), ALU.is_ge)
                b.reduce(cnt[:, pi, :], cmpb[:, ta:tb_, :].rearrange("p t e -> p e t"))
            b.mm(pcn[:].rearrange("p a e -> p (a e)"), ones_f[:], cnt[:].rearrange("p a e -> p (a e)"))
            for pi, (ta, tb_, cap) in enumerate(parts):
                b.ts(gef[:, pi, :], pcn[:, pi, :], float(cap) - 0.5, op0=ALU.is_ge)
            b.tt(dlt[:], mid[:], lo[:], ALU.subtract)
            b.tt(dlt[:], dlt[:], gef[:], ALU.mult)
            b.tt(lo[:], lo[:], dlt[:], ALU.add)
            b.tt(dlt[:], hi[:], mid[:], ALU.subtract)
            b.tt(dlt[:], dlt[:], gef[:], ALU.mult)
            b.tt(hi[:], mid[:], dlt[:], ALU.add)
        for pi, (ta, tb_, cap) in enumerate(parts):
            n_ = tb_ - ta
            b.tt(cmpb[:, ta:tb_, :], aff2[:, ta:tb_, :], lo[:, pi, :].unsqueeze(1).broadcast_to([128, n_, NE]), ALU.is_ge)
        b.tt(gm[:], cmpb[:], aff2[:], ALU.mult)
        if "GM" in cfg.debug:
            o = b.out("dbg_GM", [128, NT, NE])
            b.dma(o, gm[:])

        BIG = 100000.0
        CAPC, CAPL = parts[0][2], parts[1][2]
        CAPLP = ((CAPL + 127) // 128) * 128
        tri = b.sb("mo_tri", [128, 128])
        b.dma(tri[:], masks_in[0])
        incl = b.sb("mo_incl", [128, NT, NE])
        totb = b.sb("mo_totb", [128, NT, NE])
        te = b.sb("mo_te", [128, NE, NT])
        rst = b.sb("mo_rst", [128, NE, NT])
        cum = b.sb("mo_cum", [128, NE, NT])
        idx = P_idx
        ppx = [b.ps("mo_ppx%d" % i, [128, 512]) for i in range(2)]
        cf = cmpb[:].rearrange("p t e -> p (t e)")
        inf_ = incl[:].rearrange("p t e -> p (t e)")
        tof = totb[:].rearrange("p t e -> p (t e)")
        for c0 in range(0, NT * NE, 512):
            cw = min(512, NT * NE - c0)
            b.mm(ppx[0][:, :cw], tri[:], cf[:, c0:c0 + cw])
            b.copy(inf_[:, c0:c0 + cw], ppx[0][:, :cw])
            b.mm(ppx[1][:, :cw], ones_f[:], cf[:, c0:c0 + cw])
            b.copy(tof[:, c0:c0 + cw], ppx[1][:, :cw], eng="act")
        b.copy(te[:], totb[:].rearrange("p t e -> p e t"))
        b.memset(rst[:], 1.0)
        b.memset(rst[:, :, 0:1], 0.0)
        b.memset(rst[:, :, CT:CT + 1], 0.0)
        b.scan(cum[:].rearrange("p e t -> p (e t)"), rst[:].rearrange("p e t -> p (e t)"), te[:].rearrange("p e t -> p (e t)"), 0.0)
        b.tt(cum[:], cum[:], te[:], ALU.subtract)
        b.tt(incl[:], incl[:], cmpb[:], ALU.subtract)
        b.tt(incl[:], incl[:], cum[:].rearrange("p e t -> p t e"), ALU.add)
        for (ta_, tb2_, J) in ((0, CT, 127.0), (CT, NT, float(CAPLP))):
            b.ts(incl[:, ta_:tb2_, :], incl[:, ta_:tb2_, :], -J, op0=ALU.add)
            b.tt(incl[:, ta_:tb2_, :], incl[:, ta_:tb2_, :], cmpb[:, ta_:tb2_, :], ALU.mult)
            b.ts(incl[:, ta_:tb2_, :], incl[:, ta_:tb2_, :], J, op0=ALU.add)
        b.copy(idx[:], incl[:].rearrange("p t e -> p (t e)"))
        b.dma(IDXd, idx[:])
        b.dma(GMd, gm[:])
        zt = b.sb("mo_zt", [128, D], BF16)
        b.memset(zt[:], 0.0)
        b.memset(P_gb[0][:], 0.0)
        for e in range(NE):
            b.dma(XSc[e], zt[:])
            b.dma(XSl[e][CAPLP:CAPLP + 128, :], zt[:])
            b.dma(Yl[e][CAPLP:CAPLP + 128, :], P_gb[0][:])
            for r0 in range(0, CAPLP, 128):
                b.dma(XSl[e][r0:r0 + 128, :], zt[:], eng="act" if (r0 // 128) % 2 else "sp")
        h2t = P_h2t
        for t in range(NT):
            h_ = h2t[0]
            b.dma(h_[:], H2tok[t * 128:(t + 1) * 128, :])
            for e in range(NE):
                tgt = XSc[e] if t < CT else XSl[e]
                cap = CAPC if t < CT else CAPL
                ic = idx[:, t * NE + e:t * NE + e + 1]
                S.add("pool", lambda en, tgt=tgt, ic=ic, h_=h_, cap=cap: en.indirect_dma_start(
                    out=tgt, out_offset=bass.IndirectOffsetOnAxis(ap=ic, axis=0), in_=h_[:], in_offset=None),
                    reads=[h_[:], ic], writes=[tgt], is_dma=True)
        wg = b.sb("mo_wg", [128, KC, FF], BF16)
        wu = b.sb("mo_wu", [128, KC, FF], BF16)
        wd = b.sb("mo_wd", [128, FF // 128, D], BF16)
        xr = [b.sb("mo_xr%d" % i, [128, D], BF16) for i in range(2)]
        xsT = b.sb("mo_xsT", [128, KC, 512], BF16)
        hid = b.sb("mo_hid", [128, FF // 128, 512], BF16)
        sg = [b.sb("mo_sg%d" % i, [128, 512]) for i in range(2)]
        ysb2 = [b.sb("mo_y%d" % i, [128, D]) for i in range(2)]
        pgu = [b.ps("mo_pgu%d" % i, [128, 512]) for i in range(2)]
        pd4 = ppx
        ptx = [b.ps("mo_ptx%d" % i, [128, 8, 128], BF16) for i in range(2)]
        rgroups = [("c", 0, 128)] + [("l", r0, min(512, CAPLP - r0)) for r0 in range(0, CAPLP, 512)]
        for e in range(NE):
            b.dma(wg[:], exp_gate[l, e].rearrange("(k p) n -> p k n", p=128), eng="pool")
            b.dma(wu[:], exp_up[l, e].rearrange("(k p) n -> p k n", p=128), eng="pool")
            b.dma(wd[:], exp_down[l, e].rearrange("(k p) n -> p k n", p=128), eng="pool")
            for (pk, r0, gl_) in rgroups:
                src = XSc[e] if pk == "c" else XSl[e]
                dstY = Yc[e] if pk == "c" else Yl[e]
                for j in range(gl_ // 128):
                    x_ = xr[j % 2]
                    b.dma(x_[:], src[r0 + j * 128:r0 + (j + 1) * 128, :], eng="sp" if j % 2 else "act")
                    for half in range(2):
                        for k in range(8):
                            b.tr(ptx[half][:, k, :], x_[:, (half * 8 + k) * 128:(half * 8 + k + 1) * 128], ident_b[:])
                        b.copy(xsT[:, half * 8:(half + 1) * 8, j * 128:(j + 1) * 128], ptx[half][:], eng="act" if half else "dve")
                for fc in range(FF // 128):
                    pg_, pu_ = pgu[0], pgu[1]
                    for k in range(KC):
                        b.mm(pg_[:, :gl_], wg[:, k, fc * 128:(fc + 1) * 128], xsT[:, k, :gl_], start=(k == 0), stop=(k == KC - 1))
                    for k in range(KC):
                        b.mm(pu_[:, :gl_], wu[:, k, fc * 128:(fc + 1) * 128], xsT[:, k, :gl_], start=(k == 0), stop=(k == KC - 1))
                    s_ = sg[fc % 2]
                    b.act(s_[:, :gl_], pg_[:, :gl_], AF.Silu)
                    b.tt(hid[:, fc, :gl_], s_[:, :gl_], pu_[:, :gl_], ALU.mult)
                for j in range(gl_ // 128):
                    y_ = ysb2[j % 2]
                    for n4 in range(4):
                        p_ = pd4[n4 % 2]
                        for fc in range(FF // 128):
                            b.mm(p_[:], hid[:, fc, j * 128:(j + 1) * 128], wd[:, fc, n4 * 512:(n4 + 1) * 512], start=(fc == 0), stop=(fc == FF // 128 - 1))
                        b.copy(y_[:, n4 * 512:(n4 + 1) * 512], p_[:], eng="act" if n4 % 2 else "dve")
                    b.dma(dstY[r0 + j * 128:r0 + (j + 1) * 128, :], y_[:])
        end_scope(st7)
        st8 = scope()
        gml = b.sb("fx_g", [128, 2, D])
        for w in range(2):
            b.dma(gml[:, w, :], bcast_row(modrow(l, w, 5)))
        idx2 = P_idx
        gm2 = b.sb("fx_gm", [128, NT, NE])
        b.dma(gm2[:], GMd)
        xa = [b.sb("fx_x%d" % i, [128, D]) for i in range(2)]
        acc = [b.sb("fx_a%d" % i, [128, D]) for i in range(2)]
        gb = P_gb
        n_g = 0
        for t in range(NT):
            rows = slice(t * 128, (t + 1) * 128)
            w = 1 if t < CT else 0
            a_ = acc[t % 2]
            b.dma(xa[t % 2][:], Xcur[rows, :], eng="sp")
            b.memset(a_[:], 0.0)
            for e in range(NE):
                g_ = gb[0]
                n_g += 1
                srcY = Yc[e] if t < CT else Yl[e]
                cap = CAPC if t < CT else CAPL
                ic = idx2[:, t * NE + e:t * NE + e + 1]
                b.memset(g_[:], 0.0, eng="pool" if e % 2 else "dve")
                S.add("pool", lambda en, srcY=srcY, ic=ic, g_=g_, cap=cap: en.indirect_dma_start(
                    out=g_[:], out_offset=None, in_=srcY, in_offset=bass.IndirectOffsetOnAxis(ap=ic, axis=0)),
                    reads=[srcY, ic], writes=[g_[:]], is_dma=True)
                b.stt(a_[:], g_[:], gm2[:, t, e:e + 1], a_[:], ALU.mult, ALU.add)
            b.tt(a_[:], a_[:], gml[:, w, :], ALU.mult)
            b.tt(xa[t % 2][:], xa[t % 2][:], a_[:], ALU.add, eng="pool")
            b.dma(Xcur[rows, :], xa[t % 2][:])
        end_scope(st8)
        debug_out("Xout", Xcur, [TOK, D])

    if cfg.stop_after is None:
        st9 = scope()
        y_out = b.out("y_out", [cfg.L, D])
        fg = b.sb("fn_g", [128, D])
        b.dma(fg[:], final_g.broadcast_to([128, D]))
        xf = [b.sb("fn_x%d" % i, [128, D]) for i in range(2)]
        jk = b.sb("fn_jk", [128, D])
        sf = b.sb("fn_s", [128, 2])
        for t in range(CT, NT):
            x_ = xf[t % 2]
            sc = sf[:, t % 2:t % 2 + 1]
            b.dma(x_[:], Xcur[t * 128:(t + 1) * 128, :], eng="sp" if t % 2 else "act")
            b.memset(sc, 0.0, eng="dve")
            b.act(jk[:], x_[:], AF.Square, accum_out=sc)
            b.rstd(sc, sc, 1.0 / D, EPS)
            b.stt(x_[:], x_[:], sc, fg[:], ALU.mult, ALU.mult)
            b.dma(y_out[(t - CT) * 128:(t - CT + 1) * 128, :], x_[:])
        end_scope(st9)

    S.finish("sp")
    S.emit()
    return b


def _rope_tables(L):
    t = np.arange(L, dtype=np.float32)
    row = np.floor(t / GRID_W).astype(np.float32)
    col = (t - row * GRID_W).astype(np.float32)
    freqs = (np.float32(10000.0) ** (-np.arange(32, dtype=np.float32) / np.float32(32))).astype(np.float32)
    ar = (row[:, None] * freqs).astype(np.float32)
    ac = (col[:, None] * freqs).astype(np.float32)
    cs = np.concatenate([np.cos(ar), np.cos(ar), np.cos(ac), np.cos(ac)], axis=1).astype(np.float32)
    sn = np.concatenate([-np.sin(ar), np.sin(ar), -np.sin(ac), np.sin(ac)], axis=1).astype(np.float32)
    return cs, sn


def _masks():
    s = np.arange(128)[:, None]
    t = np.arange(128)[None, :]
    m = np.zeros((6, 128, 128), np.float32)
    m[0] = (s <= t)
    m[1] = (s >= t)
    m[2] = -1.0 * (s < t)
    m[3] = -1.0 * (s > t)
    m[4] = (s <= t)
    m[5] = (s >= t)
    return m


def prep_inputs(inputs, cfg, n_cores=2):
    f = lambda a: np.ascontiguousarray(np.asarray(a), dtype=np.float32)
    TOK, dl, L = cfg.TOK, cfg.depth, cfg.L
    x, c, ctx, c_ctx = f(inputs["x"]), f(inputs["c"]), f(inputs["ctx"]), f(inputs["c_ctx"])
    cs, sn = _rope_tables(L)
    rc = np.ones((TOK, 128), np.float32)
    rs = np.zeros((TOK, 128), np.float32)
    rc[CT * 128:] = cs
    rs[CT * 128:] = sn
    shared = {
        "mod_w": f(inputs["mod_w"][:dl]),
        "modb": f(inputs["mod_b"][:dl])[:, None, :],
        "norm1_g": f(inputs["norm1_g"][:dl])[:, None, :],
        "norm2_g": f(inputs["norm2_g"][:dl])[:, None, :],
        "final_g": f(inputs["final_g"])[None, :],
        "w_in": f(inputs["w_in"][:dl]),
        "ropeCS": rc, "ropeSN": rs,
        "attn_qn": f(inputs["attn_qn"][:dl])[:, None, :],
        "attn_kn": f(inputs["attn_kn"][:dl])[:, None, :],
        "ident": np.eye(128, dtype=np.float32),
        "masks": _masks(),
        "rw_conv": f(inputs["rwkv_conv"][:dl])[:, :, None, :],
        "rw_w0": f(inputs["rwkv_w0"][:dl]).reshape(dl, 1, 1024),
        "rw_a0": f(inputs["rwkv_a0"][:dl]).reshape(dl, 1, 1024),
        "rw_w2": f(inputs["rwkv_w2"][:dl]).reshape(dl, 128, 512),
        "rw_a2": f(inputs["rwkv_a2"][:dl]).reshape(dl, 128, 512),
        "rw_g2": f(inputs["rwkv_g2"][:dl]),
        "rw_kk": f(inputs["rwkv_kk"][:dl])[:, None, :],
        "rw_ka": f(inputs["rwkv_ka"][:dl])[:, None, :],
        "rw_rk": f(inputs["rwkv_rk"][:dl]).reshape(dl, 1, 512),
        "rw_lng": f(inputs["rwkv_ln_g"][:dl])[:, None, :],
        "rw_lnb": f(inputs["rwkv_ln_b"][:dl])[:, None, :],
    }
    def s5_pn(a):
        a = f(a)[:dl].reshape(dl, 2, 16, 2, 64)
        return np.ascontiguousarray(a.transpose(0, 3, 4, 1, 2).reshape(dl, 128, 2, 16))
    ldt = np.broadcast_to(f(inputs["s5_log_dt"])[:dl][:, :, :, None], (dl, 2, 32, 64))
    shared["s5_par"] = np.ascontiguousarray(np.stack([s5_pn(inputs["s5_lam_re"]), s5_pn(inputs["s5_lam_im"]), s5_pn(ldt)], axis=2))
    def s5_bd(a, bmat):
        a = f(a)[:dl]
        if not bmat:
            a = a.transpose(0, 1, 2, 4, 3)
        a = a.reshape(dl, 2, 16, 2, 64, 16)
        o = np.zeros((dl, 2, 64, 2, 16, 2, 16), np.float32)
        for gl in range(2):
            o[:, gl, :, :, :, gl, :] = a[:, :, :, gl].transpose(0, 3, 1, 2, 4)
        return o.reshape(dl, 128, 2, 16, 32)
    shared["s5_bmat"] = np.ascontiguousarray(np.stack([s5_bd(inputs["s5_b_re"], True), s5_bd(inputs["s5_b_im"], True)], axis=2))
    shared["s5_cmat"] = np.ascontiguousarray(np.stack([s5_bd(inputs["s5_c_re"], False), s5_bd(inputs["s5_c_im"], False)], axis=2))
    shared["s5_glu"] = f(inputs["s5_glu"][:dl])
    shared["s5_dskip"] = np.ascontiguousarray(f(inputs["s5_d"])[:dl].reshape(dl, 16, 32).transpose(0, 2, 1))
    shared["iota512"] = np.arange(512, dtype=np.float32)[None, :]
    for k in ("w_branch", "w_out", "router", "exp_gate", "exp_up", "exp_down"):
        shared[k] = f(inputs[k][:dl])
    maps = []
    for bi in range(n_cores):
        m = dict(shared)
        m["x_seq"] = np.concatenate([ctx[bi], x[bi]], axis=0)
        cv = np.stack([c[bi], c_ctx], 0)
        m["csT"] = np.ascontiguousarray(cv.reshape(2, D // 128, 128).transpose(2, 1, 0))
        maps.append(m)
    return maps


_CACHE = {}


def kernel(**inputs):
    cfg = Cfg()
    if "b" not in _CACHE:
        _CACHE["b"] = build(cfg)
    b = _CACHE["b"]
    maps = prep_inputs(inputs, cfg, n_cores=2)
    maps = [{k: v for k, v in m.items() if k in b.ins} for m in maps]
    res = run_bass_kernel_spmd(b.nc, maps, core_ids=[0, 1])
    out = np.stack([np.asarray(res.results[i]["y_out"], dtype=np.float32) for i in range(2)], axis=0)
    return out
```
